# Optimizing a Trainium2 kernel written in Bass

```python
import math
import jax, jax.numpy as jnp
from jax import lax
import numpy as np

D_MODEL = 1024
BATCH = 16
SEQ = 2048
DEPTH = 1

CTX_LEN = 256
GRID_W = 64
MIX_WIDTH = D_MODEL
HY_WIDTH = MIX_WIDTH // 2
HG_WIDTH = MIX_WIDTH - HY_WIDTH
HG_HEADS = 4
HG_HEAD_DIM = HG_WIDTH // HG_HEADS
HY_ORDER = 2
HY_SHORT = 3
HY_BANDS = 16
HY_POS_DIM = 2 * HY_BANDS + 1
HY_FILTER_HIDDEN = 64
HY_DECAY_TARGET = 1e-2
HY_FAST_DECAY = 0.3
HY_SLOW_DECAY = 1.5
HY_WINDOW_SHIFT = 0.05
CHUNK = 64
N_EXPERTS = 16
CAPACITY_FACTOR = 2
EXPERT_FF = 2 * D_MODEL
IN_COLS = 3 * HY_WIDTH + 5 * HG_WIDTH
EPS = 1e-6

kernel_name = "hyena_hgrn2_ec_moe_prefix_dit_block"


def rmsnorm(x, g):
    xf = x.astype(jnp.float32)
    y = xf * lax.rsqrt(jnp.mean(xf * xf, axis=-1, keepdims=True) + EPS)
    return (y * g.astype(jnp.float32)).astype(x.dtype)


def modulate(h, shift, scale):
    return h * (1 + scale) + shift


def short_conv(u, w, b, n_rows):
    bn, L, C = u.shape
    row_len = L // n_rows
    pad = HY_SHORT // 2
    up = jnp.pad(u.reshape(bn, n_rows, row_len, C), ((0, 0), (0, 0), (pad, pad), (0, 0)))
    y = b + sum(up[:, :, j:j + row_len] * w[j] for j in range(HY_SHORT))
    return y.reshape(bn, L, C)


def hyena_filter_freqs(L, w1, b1, fr1, w2, b2, fr2, w3):
    C = w3.shape[1] // (HY_ORDER * 2)
    t = jnp.linspace(0.0, 1.0, L, dtype=jnp.float32)[:, None]
    w = 2.0 * math.pi * jnp.arange(L, dtype=jnp.float32)[:, None] / L
    f = jnp.linspace(1e-4, HY_BANDS - 1, HY_BANDS, dtype=jnp.float32)[None, :]
    z = jnp.concatenate([t, jnp.cos(f * w), -jnp.sin(f * w)], axis=-1)
    h = jnp.sin(fr1.astype(jnp.float32) * (z @ w1.astype(jnp.float32) + b1.astype(jnp.float32)))
    h = jnp.sin(fr2.astype(jnp.float32) * (h @ w2.astype(jnp.float32) + b2.astype(jnp.float32)))
    h = (h @ w3.astype(jnp.float32)).reshape(L, HY_ORDER, 2, C)
    min_decay = math.log(HY_DECAY_TARGET) / HY_SLOW_DECAY
    max_decay = math.log(HY_DECAY_TARGET) / HY_FAST_DECAY
    deltas = jnp.abs(jnp.linspace(min_decay, max_decay, C, dtype=jnp.float32))
    window = jnp.exp(-t * deltas[None, :]) + HY_WINDOW_SHIFT
    h = h * window[:, None, None, :]
    full = jnp.concatenate([h[:, :, 0], jnp.zeros((1, HY_ORDER, C), jnp.float32), h[:0:-1, :, 1]], axis=0)
    full = full / (jnp.sum(jnp.abs(full), axis=0, keepdims=True) + EPS)
    return jnp.fft.rfft(full, axis=0)


def long_conv(u, hf, bias):
    L = u.shape[1]
    U = jnp.fft.rfft(u, n=2 * L, axis=1)
    y = jnp.fft.irfft(U * hf[None], n=2 * L, axis=1)[:, :L]
    return y + u * bias.astype(jnp.float32)


def hyena_mixer(p, n_rows, conv_w, conv_b, filt, bias):
    L = p.shape[1]
    u = short_conv(p, conv_w, conv_b, n_rows).astype(jnp.float32)
    v, x1, x2 = jnp.split(u, 3, axis=-1)
    hf = hyena_filter_freqs(L, *filt)
    z = x1 * long_conv(v, hf[:, 0], bias[0])
    y = x2 * long_conv(z, hf[:, 1], bias[1])
    return y.astype(p.dtype)


def chunk_scan(q, k, v, logf, s0):
    bn, L, H, K = q.shape
    n = L // CHUNK

    def blocks(a):
        return a.reshape(bn, n, CHUNK, H, a.shape[-1]).swapaxes(0, 1)

    earlier_or_same = jnp.tril(jnp.ones((CHUNK, CHUNK), dtype=bool))[None, :, :, None, None]

    def step(S, inp):
        qb, kb, vb, gb = inp
        G = jnp.cumsum(gb, axis=1)
        o_inter = jnp.einsum('bthk,bhkv->bthv', qb * jnp.exp(G), S)
        decay = jnp.exp(jnp.where(earlier_or_same, G[:, :, None] - G[:, None, :], -jnp.inf))
        A = jnp.einsum('bthk,bshk,btshk->bhts', qb, kb, decay)
        o_intra = jnp.einsum('bhts,bshv->bthv', A, vb)
        G_last = G[:, -1]
        S_new = jnp.exp(G_last)[..., None] * S + jnp.einsum('bshk,bshv->bhkv', kb * jnp.exp(G_last[:, None] - G), vb)
        return S_new, o_inter + o_intra

    S_fin, o = lax.scan(step, s0, (blocks(q), blocks(k), blocks(v), blocks(logf)))
    return o.swapaxes(0, 1).reshape(bn, L, H, v.shape[-1]), S_fin


def hgrn_project(p, lb):
    bn, L, _ = p.shape
    q, i, zf, zb, g = jnp.split(p, 5, axis=-1)

    def heads(a):
        return a.reshape(bn, L, HG_HEADS, HG_HEAD_DIM)

    q = heads(jax.nn.silu(q.astype(jnp.float32)) * HG_HEAD_DIM ** -0.5)
    v = heads(i.astype(jnp.float32))

    def gate(z, lb_d):
        f = lb_d + (1.0 - lb_d) * jax.nn.sigmoid(z.astype(jnp.float32))
        return heads(1.0 - f), heads(jnp.log(f))

    return q, v, gate(zf, lb[0]), gate(zb, lb[1]), g


def hgrn_readout(o, g, norm_g):
    bn, L = o.shape[:2]
    o = rmsnorm(o, norm_g).reshape(bn, L, HG_WIDTH)
    return (o * jax.nn.silu(g.astype(jnp.float32))).astype(g.dtype)


def hgrn_mixer(p_lat, p_ctx, lb, norm_g, need_ctx):
    ql, vl, (kfl, gfl), (kbl, gbl), g_lat = hgrn_project(p_lat, lb)
    qc, vc, (kfc, gfc), (kbc, gbc), g_ctx = hgrn_project(p_ctx, lb)
    s0 = jnp.zeros((p_lat.shape[0], HG_HEADS, HG_HEAD_DIM, HG_HEAD_DIM), jnp.float32)
    flip = lambda a: a[:, ::-1]
    oc_f, S_f = chunk_scan(qc, kfc, vc, gfc, s0)
    oc_b, S_b = chunk_scan(flip(qc), flip(kbc), flip(vc), flip(gbc), s0)
    ol_f, _ = chunk_scan(ql, kfl, vl, gfl, S_f)
    ol_b, _ = chunk_scan(flip(ql), flip(kbl), flip(vl), flip(gbl), S_b)
    out_lat = hgrn_readout(ol_f + flip(ol_b), g_lat, norm_g)
    out_ctx = hgrn_readout(oc_f + flip(oc_b), g_ctx, norm_g) if need_ctx else None
    return out_lat, out_ctx


def ec_moe(h, router_w, w1, w3, w2):
    bn, N, D = h.shape
    cap = CAPACITY_FACTOR * N // N_EXPERTS
    aff = jax.nn.softmax(jnp.einsum('bnd,de->bne', h, router_w).astype(jnp.float32), axis=-1)
    gates, idx = lax.top_k(jnp.swapaxes(aff, 1, 2), cap)
    xg = jax.vmap(lambda hb, ib: hb[ib])(h, idx)
    a = jnp.einsum('becd,edf->becf', xg, w1)
    b = jnp.einsum('becd,edf->becf', xg, w3)
    y = jnp.einsum('becf,efd->becd', jax.nn.silu(a) * b, w2)
    y = y * gates[..., None].astype(y.dtype)
    return jax.vmap(lambda yb, ib: jnp.zeros((N, D), yb.dtype).at[ib.reshape(-1)].add(yb.reshape(-1, D)))(y, idx)


def setup_inputs(seed: int = 0) -> dict:
    key = jax.random.key(seed)
    ks = jax.random.split(key, 28)

    def nrm(k, shape, s):
        return jax.random.normal(k, shape, jnp.float32) * s

    D, L_ = D_MODEL, DEPTH
    return {
        "x": nrm(ks[0], (BATCH, SEQ, D), 1.0),
        "c": nrm(ks[1], (BATCH, D), 1.0),
        "ctx": nrm(ks[2], (BATCH, CTX_LEN, D), 1.0),
        "c_ctx": nrm(ks[3], (D,), 1.0),
        "ada_w": nrm(ks[4], (L_, D, 6 * D), 0.5 * D ** -0.5),
        "ada_b": nrm(ks[5], (L_, 6 * D), 0.02),
        "norm1_g": 1.0 + nrm(ks[6], (L_, D), 0.05),
        "w_in": nrm(ks[7], (L_, D, IN_COLS), D ** -0.5),
        "hy_conv_w": nrm(ks[8], (L_, HY_SHORT, 3 * HY_WIDTH), HY_SHORT ** -0.5),
        "hy_conv_b": nrm(ks[9], (L_, 3 * HY_WIDTH), 0.02),
        "hy_pos_w1": nrm(ks[10], (L_, HY_POS_DIM, HY_FILTER_HIDDEN), HY_POS_DIM ** -0.5),
        "hy_pos_b1": nrm(ks[11], (L_, HY_FILTER_HIDDEN), 0.1),
        "hy_freq1": 1.0 + nrm(ks[12], (L_, HY_FILTER_HIDDEN), 0.1),
        "hy_pos_w2": nrm(ks[13], (L_, HY_FILTER_HIDDEN, HY_FILTER_HIDDEN), HY_FILTER_HIDDEN ** -0.5),
        "hy_pos_b2": nrm(ks[14], (L_, HY_FILTER_HIDDEN), 0.1),
        "hy_freq2": 1.0 + nrm(ks[15], (L_, HY_FILTER_HIDDEN), 0.1),
        "hy_pos_w3": nrm(ks[16], (L_, HY_FILTER_HIDDEN, HY_ORDER * 2 * HY_WIDTH), HY_FILTER_HIDDEN ** -0.5),
        "hy_bias": nrm(ks[17], (L_, HY_ORDER, HY_WIDTH), 0.5),
        "hy_norm_g": 1.0 + nrm(ks[18], (L_, HY_WIDTH), 0.05),
        "hg_lb": nrm(ks[19], (L_ + 1, 2, HG_WIDTH), 1.0),
        "hg_norm_g": 1.0 + nrm(ks[20], (L_, HG_HEAD_DIM), 0.05),
        "w_out": nrm(ks[21], (L_, MIX_WIDTH, D), MIX_WIDTH ** -0.5),
        "norm2_g": 1.0 + nrm(ks[22], (L_, D), 0.05),
        "router_w": nrm(ks[23], (L_, D, N_EXPERTS), D ** -0.5),
        "exp_w1": nrm(ks[24], (L_, N_EXPERTS, D, EXPERT_FF), D ** -0.5),
        "exp_w3": nrm(ks[25], (L_, N_EXPERTS, D, EXPERT_FF), D ** -0.5),
        "exp_w2": nrm(ks[26], (L_, N_EXPERTS, EXPERT_FF, D), EXPERT_FF ** -0.5),
        "final_g": 1.0 + nrm(ks[27], (D,), 0.05),
    }


def reference(x, c, ctx, c_ctx, ada_w, ada_b, norm1_g, w_in, hy_conv_w, hy_conv_b, hy_pos_w1, hy_pos_b1,
              hy_freq1, hy_pos_w2, hy_pos_b2, hy_freq2, hy_pos_w3, hy_bias, hy_norm_g, hg_lb, hg_norm_g,
              w_out, norm2_g, router_w, exp_w1, exp_w3, exp_w2, final_g):
    rows = x.shape[1] // GRID_W
    lb_all = jnp.cumsum(jax.nn.softmax(hg_lb.astype(jnp.float32), axis=0), axis=0)
    xc = ctx
    s_lat = jax.nn.silu(c)
    s_ctx = jax.nn.silu(c_ctx)
    for l in range(DEPTH):
        last = l == DEPTH - 1
        sh1, sc1, g1, sh2, sc2, g2 = jnp.split((s_lat @ ada_w[l] + ada_b[l])[:, None, :], 6, axis=-1)
        sh1c, sc1c, g1c, sh2c, sc2c, g2c = jnp.split(s_ctx @ ada_w[l] + ada_b[l], 6, axis=-1)

        h = modulate(rmsnorm(x, norm1_g[l]), sh1, sc1)
        hc = modulate(rmsnorm(xc, norm1_g[l]), sh1c, sc1c)
        p = h @ w_in[l]
        pc = hc @ w_in[l]
        filt = (hy_pos_w1[l], hy_pos_b1[l], hy_freq1[l], hy_pos_w2[l], hy_pos_b2[l], hy_freq2[l], hy_pos_w3[l])
        y_hy = hyena_mixer(p[..., :3 * HY_WIDTH], rows, hy_conv_w[l], hy_conv_b[l], filt, hy_bias[l])
        y_hg, y_hg_c = hgrn_mixer(p[..., 3 * HY_WIDTH:], pc[..., 3 * HY_WIDTH:], lb_all[l], hg_norm_g[l], not last)
        mix = jnp.concatenate([rmsnorm(y_hy, hy_norm_g[l]).astype(x.dtype), y_hg.astype(x.dtype)], axis=-1) @ w_out[l]
        x = x + g1 * mix
        if not last:
            y_hy_c = hyena_mixer(pc[..., :3 * HY_WIDTH], 1, hy_conv_w[l], hy_conv_b[l], filt, hy_bias[l])
            mix_c = jnp.concatenate([rmsnorm(y_hy_c, hy_norm_g[l]).astype(xc.dtype), y_hg_c.astype(xc.dtype)], axis=-1) @ w_out[l]
            xc = xc + g1c * mix_c

        h2 = modulate(rmsnorm(x, norm2_g[l]), sh2, sc2)
        x = x + g2 * ec_moe(h2, router_w[l], exp_w1[l], exp_w3[l], exp_w2[l])
        if not last:
            h2c = modulate(rmsnorm(xc, norm2_g[l]), sh2c, sc2c)
            xc = xc + g2c * ec_moe(h2c, router_w[l], exp_w1[l], exp_w3[l], exp_w2[l])
    return rmsnorm(x, final_g)
```

```python
import math
import os
from contextlib import ExitStack

import numpy as np
import ml_dtypes

import concourse.bass as bass
import concourse.mybir as mybir
from concourse.bass_utils import run_bass_kernel_spmd

F32 = mybir.dt.float32
BF16 = mybir.dt.bfloat16
ALU = mybir.AluOpType
AF = mybir.ActivationFunctionType
AX = mybir.AxisListType

EPS = 1e-6
NCORES = 8
NB = 2
L = 2048
D = 1024
CTX = 256
NE = 16
CAP = 256
PI = math.pi


class Sched:
    R = 16000
    ND = 16

    def __init__(self, nc, stack):
        self.nc = nc
        self.e = dict(pe=nc.tensor, dve=nc.vector, act=nc.scalar, pool=nc.gpsimd, sp=nc.sync)
        self.csem = {k: [] for k in self.e}
        self.stack = stack
        self.cnt = {k: 0 for k in self.e}
        self.seen = {k: {} for k in self.e}
        self.lastw = {}
        self.readers = {}
        self.dsem = {q: [stack.enter_context(nc.semaphore(f"d_{q}_{i}")) for i in range(self.ND)]
                     for q in ('sp', 'pool')}
        self.duse = {q: [0] * self.ND for q in self.dsem}
        self.dnext = {q: 0 for q in self.dsem}
        self.nwait = 0
        self.ninst = 0

    def _csem(self, eng, idx):
        j = idx // self.R
        while len(self.csem[eng]) <= j:
            self.csem[eng].append(self.stack.enter_context(
                self.nc.semaphore(f"c_{eng}_{len(self.csem[eng])}")))
        return self.csem[eng][j], idx % self.R + 1

    def _deps(self, reads, writes):
        toks = []
        for k in reads:
            w = self.lastw.get(k)
            if w is not None:
                toks.append(w)
        for k in writes:
            w = self.lastw.get(k)
            if w is not None:
                toks.append(w)
            r = self.readers.get(k)
            if r:
                toks.extend(r.values())
        return toks

    def _wait(self, eng, toks):
        best = {}
        for (teng, sem, val) in toks:
            if teng == 'pe' and eng == 'pe':
                continue
            sid = id(sem)
            if val > best.get(sid, (None, 0))[1]:
                best[sid] = (sem, val)
        seen = self.seen[eng]
        for sid, (sem, val) in best.items():
            if seen.get(sid, 0) >= val:
                continue
            self.e[eng].wait_ge(sem, val)
            self.nwait += 1
            seen[sid] = val

    def _reg(self, tok, reads, writes):
        sid = id(tok[1])
        for k in reads:
            self.readers.setdefault(k, {})[sid] = tok
        for k in writes:
            self.lastw[k] = tok
            self.readers[k] = {}

    def op(self, eng, fn, reads=(), writes=()):
        self._wait(eng, self._deps(reads, writes))
        inst = fn(self.e[eng])
        idx = self.cnt[eng]
        self.cnt[eng] += 1
        sem, val = self._csem(eng, idx)
        inst.then_inc(sem, 1)
        self.ninst += 1
        tok = (eng, sem, val)
        self._reg(tok, reads, writes)
        return tok

    def dma(self, q, out, in_, reads=(), writes=(), **kw):
        toks = self._deps(reads, writes)
        j = self.dnext[q]
        self.dnext[q] = (j + 1) % self.ND
        sem = self.dsem[q][j]
        uses = self.duse[q][j]
        if uses > 0:
            toks.append(('dma', sem, 16 * uses))
        self._wait(q, toks)
        inst = self.e[q].dma_start(out=out, in_=in_, **kw)
        inst.then_inc(sem, 16)
        self.ninst += 1
        self.duse[q][j] = uses + 1
        tok = ('dma', sem, 16 * (uses + 1))
        self._reg(tok, reads, writes)
        return tok

    def barrier(self):
        toks = []
        for eng in self.e:
            if self.cnt[eng] > 0:
                sem, val = self._csem(eng, self.cnt[eng] - 1)
                toks.append((eng + '_b', sem, val))
        for q in self.dsem:
            for j in range(self.ND):
                if self.duse[q][j] > 0:
                    toks.append(('dma', self.dsem[q][j], 16 * self.duse[q][j]))
        for eng in self.e:
            self._wait(eng, toks)


class K:
    pass


class Phase:
    def __init__(self, k, name):
        self.k = k
        self.name = name
        self.st = ExitStack()
        self.n = 0

    def __enter__(self):
        self.st.__enter__()
        return self

    def __exit__(self, *a):
        self.k.S.barrier()
        return self.st.__exit__(*a)

    def tile(self, shape, dt, name=None):
        self.n += 1
        nm = f"{self.name}_{name or 't'}_{self.n}"
        return self.st.enter_context(self.k.nc.sbuf_tensor(nm, list(shape), dt))

    def ring(self, n, shape, dt, name):
        return Ring([(self.tile(shape, dt, f"{name}{i}"), f"{self.name}.{name}{i}") for i in range(n)])


class Ring:
    def __init__(self, items):
        self.items = items
        self.i = 0

    def next(self):
        it = self.items[self.i % len(self.items)]
        self.i += 1
        return it


def mmgroup(S, out, pairs, reads, wkeys):
    def fn(e):
        n = len(pairs)
        inst = None
        for i, (l, r) in enumerate(pairs):
            inst = e.matmul(out, l, r, start=(i == 0), stop=(i == n - 1))
        return inst
    return S.op('pe', fn, reads=reads, writes=wkeys)


def bc_row(ap_row, n, parts=128):
    return ap_row.to_broadcast([parts, n])


_CONST_CACHE = {}


def make_consts():
    if _CONST_CACHE:
        return _CONST_CACHE
    bf = ml_dtypes.bfloat16
    N = 2 * L
    s = np.arange(L, dtype=np.float64)[:, None]
    f = np.arange(L, dtype=np.float64)[None, :] + 0.5
    ang = (2.0 * np.pi / N) * s * f
    C = np.cos(ang)
    Sn = np.sin(ang)

    def lay(M):
        return np.ascontiguousarray(M.reshape(16, 128, 16, 128).transpose(2, 1, 0, 3))

    c = {}
    c["tabF"] = np.stack([lay(C), lay(Sn)]).astype(bf)
    c["tabI"] = np.stack([lay(C.T), lay(Sn.T)]).astype(bf)
    t = np.linspace(0.0, 1.0, L, dtype=np.float32)[:, None]
    w = (2.0 * np.float32(math.pi) * np.arange(L, dtype=np.float32)[:, None] / np.float32(L)).astype(np.float32)
    fb = np.linspace(1e-4, 15, 16, dtype=np.float32)[None, :]
    z = np.concatenate([t, np.cos(fb * w), -np.sin(fb * w)], axis=-1).astype(np.float32)
    c["zT"] = np.ascontiguousarray(z.T)
    min_decay = math.log(1e-2) / 1.5
    max_decay = math.log(1e-2) / 0.3
    deltas = np.abs(np.linspace(min_decay, max_decay, 512, dtype=np.float32))
    window = (np.exp(-t * deltas[None, :]) + np.float32(0.05)).astype(np.float32)
    c["win"] = np.ascontiguousarray(window.reshape(16, 128, 512).transpose(1, 0, 2))
    idx = np.arange(128)
    ss, tt = idx[:, None], idx[None, :]
    m128 = {}
    m128["ident"] = (ss == tt)
    m128["ones"] = np.ones((128, 128))
    m128["shl"] = (ss == tt - 1) & (tt % 64 != 0)
    m128["shr"] = (ss == tt + 1) & (tt % 64 != 63)
    same = (ss // 64) == (tt // 64)
    anch = (tt // 64) * 64 + 31
    m128["cumf"] = (same & (ss <= tt)).astype(np.float64) - (same & (ss <= anch)).astype(np.float64)
    m128["cumb"] = (same & (ss >= tt)).astype(np.float64) - (same & (ss >= anch)).astype(np.float64)
    m128["maskf"] = same & (ss <= tt)
    m128["maskb"] = same & (ss >= tt)
    m128["tri"] = ss < tt
    names = ["ident", "ones", "shl", "shr", "cumf", "cumb", "maskf", "maskb", "tri"]
    c["m128"] = np.stack([m128[n].astype(np.float32) for n in names])
    c["m128_names"] = names
    sel = np.zeros((2, 128, 4), np.float32)
    for j in range(2):
        inch = (idx // 64) == j
        a = j * 64 + 31
        sel[0, :, 2 * j] = inch & (idx <= a)
        sel[0, :, 2 * j + 1] = inch
        sel[1, :, 2 * j] = inch & (idx >= a)
        sel[1, :, 2 * j + 1] = inch
    c["sel"] = sel
    small = np.zeros((128, 260), np.float32)
    small[:, 0:256] = np.arange(256)[None, :]
    small[:, 256] = idx
    small[:, 257] = idx + 128
    small[:, 258] = (idx < 64)
    small[:, 259] = (idx >= 64)
    c["small"] = small
    _CONST_CACHE.update(c)
    return _CONST_CACHE


def build_nc(debug=False, stop_after=None):
    nc = bass.Bass("TRN2", target_bir_lowering=False)
    k = K()
    k.nc = nc
    k.debug = debug
    k.dbg_out = []

    def din(name, shape, dt=F32):
        return nc.dram_tensor(name, list(shape), dt, kind="ExternalInput").ap()

    def dscr(name, shape, dt=F32):
        kind = "ExternalOutput" if debug else "Internal"
        if debug:
            k.dbg_out.append(name)
        return nc.dram_tensor(name, list(shape), dt, kind=kind).ap()

    k.x = din("x", [NB * L, D])
    k.ctx = din("ctx", [NB * CTX, D])
    k.clay = din("clay", [128, 8, 3])
    k.ada_w = din("ada_w", [D, 6 * D])
    k.ada_b = din("ada_b", [1, 6 * D])
    k.norm1_g = din("norm1_g", [1, D])
    k.w_in = din("w_in", [D, 4096])
    k.hy_conv_w = din("hy_conv_w", [3, 1536])
    k.hy_conv_b = din("hy_conv_b", [1, 1536])
    k.pos_w1 = din("hy_pos_w1", [33, 64])
    k.pos_b1 = din("hy_pos_b1", [64, 1])
    k.freq1 = din("hy_freq1", [64, 1])
    k.pos_w2 = din("hy_pos_w2", [64, 64])
    k.pos_b2 = din("hy_pos_b2", [64, 1])
    k.freq2 = din("hy_freq2", [64, 1])
    k.pos_w3 = din("hy_pos_w3", [64, 2048])
    k.hy_bias = din("hy_bias", [2, 512])
    k.hy_norm_g = din("hy_norm_g", [1, 512])
    k.hg_lb = din("hg_lb", [2, 1024])
    k.hg_norm_g = din("hg_norm_g", [1, 128])
    k.w_out = din("w_out", [D, D])
    k.norm2_g = din("norm2_g", [1, D])
    k.router_w = din("router_w", [D, NE])
    k.exp_w1 = din("exp_w1", [NE, D, 2 * D])
    k.exp_w3 = din("exp_w3", [NE, D, 2 * D])
    k.exp_w2 = din("exp_w2", [NE, 2 * D, D])
    k.final_g = din("final_g", [1, D])
    k.tabF = din("tabF", [2, 16, 128, 16 * 128], BF16)
    k.tabI = din("tabI", [2, 16, 128, 16 * 128], BF16)
    k.zT = din("zT", [33, L])
    k.win = din("win", [128, 16, 512])
    k.m128 = din("m128", [9, 128, 128])
    k.sel = din("sel", [2, 128, 4])
    k.small = din("small", [128, 260])
    k.out = nc.dram_tensor("out", [NB * L, D], F32, kind="ExternalOutput").ap()
    k.modv = dscr("modv", [3, 6 * D])
    k.U = dscr("U", [NB * L, 1536])
    k.Phg = dscr("Phg", [NB * L, 2560])
    k.Pctx = dscr("Pctx", [NB * CTX, 1536])
    k.Hs = dscr("Hs", [2, 16, 128, 1024])
    k.Ymix = dscr("Ymix", [NB * L, D], BF16)
    k.Of = dscr("Of", [NB * L, 512])
    k.Xmid = dscr("Xmid", [NB * L, D])
    k.H2 = dscr("H2", [NB * L, D], BF16)
    k.XG = dscr("XG", [NE, 128, 8, NB * CAP], BF16)
    k.Y = dscr("Y", [NB, 128, 2 * NE, D], BF16)
    if debug:
        k.dbg_aff = dscr("dbg_aff", [128, NB * 16 * NE])
        k.dbg_val = dscr("dbg_val", [128, NB * 16 * NE])

    stages = ["P", "F", "A", "H", "G", "O", "R", "XG", "FF", "SC"]
    if stop_after is not None:
        stages = stages[:stages.index(stop_after) + 1]
    import os
    if os.environ.get("K_STAGES"):
        stages = os.environ["K_STAGES"].split(",")

    with ExitStack() as st:
        S = Sched(nc, st)
        k.S = S
        k.psum = st.enter_context(nc.psum_tensor("ps", [128, 4096], F32))
        k.bank = lambda i: k.psum[:, i * 512:(i + 1) * 512]
        with Phase(k, "G0") as g0:
            mnames = make_consts()["m128_names"]
            k.c32 = {}
            k.cbf = {}
            for i, n in enumerate(mnames):
                t32 = g0.tile([128, 128], F32, n + "32")
                S.dma('sp', t32[:], k.m128[i], writes=[n + "32"])
                k.c32[n] = t32
                tb = g0.tile([128, 128], BF16, n + "bf")
                S.dma('pool', tb[:], k.m128[i], writes=[n + "bf"])
                k.cbf[n] = tb
            k.smallt = g0.tile([128, 260], F32, "small")
            S.dma('sp', k.smallt[:], k.small, writes=["small"])
            k.aff = g0.tile([128, NB, 16, NE], F32, "aff")
            k.val = g0.tile([128, NB, 16, NE], F32, "val")
            k.affhl = g0.tile([128, NB, 16, NE, 2], BF16, "affhl")
            k.gates = g0.tile([128, NE, NB, 2], F32, "gates")
            S.barrier()
            fns = dict(P=stage_P, F=stage_F, A=stage_A, H=stage_H, G=stage_G, O=stage_O, R=stage_R,
                       XG=stage_XG, FF=stage_FF, SC=stage_SC)
            for sname in stages:
                fns[sname](k)
                S.barrier()
        S.barrier()
    k.stats = (S.ninst, S.nwait)
    return nc, k


def stage_P(k):
    S, nc = k.S, k.nc
    with Phase(k, "P") as ph:
        sT = ph.tile([128, 8, 3], F32, "sT")
        sTs = ph.tile([128, 8, 3], F32, "sTs")
        adab = ph.tile([3, 6 * D], F32, "adab")
        S.dma('sp', sT[:], k.clay, writes=["sT"])
        S.dma('sp', adab[:], bc_row(k.ada_b, 6 * D, 3), writes=["adab"])
        S.op('act', lambda e: e.activation(sTs[:], sT[:], AF.Silu), reads=["sT"], writes=["sTs"])
        wr = ph.ring(2, [128, 8, 512], F32, "w")
        rr = ph.ring(2, [3, 512], F32, "r")
        awv = k.ada_w.rearrange("(k p) n -> p k n", p=128)
        for cc in range(12):
            wt, wk = wr.next()
            S.dma('sp', wt[:], awv[:, :, cc * 512:(cc + 1) * 512], writes=[wk])
            b = cc % 2
            mmgroup(S, k.bank(b)[0:3, :], [(sTs[:, kk, :], wt[:, kk, :]) for kk in range(8)],
                    reads=["sTs", wk], wkeys=[f"ps{b}"])
            r, rk = rr.next()
            S.op('dve', lambda e: e.tensor_tensor(r[:], k.bank(b)[0:3, :], adab[:, cc * 512:(cc + 1) * 512], ALU.add),
                 reads=[f"ps{b}", "adab"], writes=[rk])
            S.dma('pool', k.modv[:, cc * 512:(cc + 1) * 512], r[:], reads=[rk], writes=["modv"])


def load_mod(k, ph, b, which, name):
    t = ph.tile([128, D], F32, name)
    k.S.dma('sp', t[:], bc_row(k.modv[b:b + 1, which * D:(which + 1) * D], D), reads=["modv"], writes=[name])
    return t


def make_scale(k, ph, b, which_sc, gain_ap, name):
    S = k.S
    sc = load_mod(k, ph, b, which_sc, name + "_sc")
    g = ph.tile([128, D], F32, name + "_g")
    S.dma('sp', g[:], bc_row(gain_ap, D), writes=[name + "_g"])
    a = ph.tile([128, D], F32, name)
    S.op('dve', lambda e: e.scalar_tensor_tensor(a[:], sc[:], 1.0, g[:], ALU.add, ALU.mult),
         reads=[name + "_sc", name + "_g"], writes=[name])
    return a


def rms_rstd(k, src, src_keys, junk, junk_key, sq, sqk, n, ncols=1):
    S = k.S
    S.op('act', lambda e: e.activation(junk, src, AF.Square, accum_out=sq[:, 0:1]),
         reads=src_keys, writes=[junk_key, sqk + "a"])
    S.op('act', lambda e: e.activation(sq[:, 1:2], sq[:, 0:1], AF.Sqrt, scale=1.0 / n, bias=EPS),
         reads=[sqk + "a"], writes=[sqk + "b"])
    S.op('dve', lambda e: e.reciprocal(sq[:, 2:3], sq[:, 1:2]), reads=[sqk + "b"], writes=[sqk])
    return sq[:, 2:3]


def stage_A(k):
    S, nc = k.S, k.nc
    KW = int(os.environ.get("A_WIN", "5"))
    with Phase(k, "A") as ph:
        win = ph.tile([128, 8, 4096], BF16, "win")
        wv = k.w_in.rearrange("(k p) n -> p k n", p=128)
        for cc in range(8):
            S.dma('pool', win[:, :, cc * 512:(cc + 1) * 512], wv[:, :, cc * 512:(cc + 1) * 512], writes=[f"win{cc}"])
        A1 = [ph.tile([128, D], F32, f"A1_{b}") for b in range(3)]
        B1 = []
        with Phase(k, "Atmp") as pt:
            for b in range(3):
                a_ = make_scale(k, pt, b, 1, k.norm1_g, f"A1t_{b}")
                S.op('pool', lambda e: e.tensor_copy(A1[b][:], a_[:]), reads=[f"A1t_{b}"], writes=[f"A1_{b}"])
        for b in range(3):
            B1.append(load_mod(k, ph, b, 0, f"B1_{b}"))
        CW = ph.tile([128, 3, 1536], F32, "CW")
        for j in range(3):
            S.dma('sp', CW[:, j, :], bc_row(k.hy_conv_w[j:j + 1, :], 1536), writes=[f"CW{j}"])
        CB = ph.tile([128, 1536], F32, "CB")
        S.dma('sp', CB[:], bc_row(k.hy_conv_b, 1536), writes=["CB"])
        xr = ph.ring(5, [128, D], F32, "x")
        junk = ph.tile([128, D], BF16, "junk")
        sqr = ph.ring(5, [128, 4], F32, "sq")
        hbr = ph.ring(3, [128, D], BF16, "hb")
        hTr = ph.ring(4, [128, 8, 128], BF16, "hT")
        pbr = ph.ring(3, [128, 1536], BF16, "pb")
        ucr = ph.ring(4, [128, 1536], F32, "uc")
        ttr = ph.ring(1, [128, 1536], F32, "tt")
        pgr = ph.ring(2, [128, 512], F32, "pg")
        ident = k.cbf["ident"]
        blocks = [(b, i, False) for b in range(NB) for i in range(2)] + [(b, i, True) for b in range(NB) for i in range(16)]
        psT = k.bank(7).bitcast(BF16)

        def unit(b, blk, lat):
            xt, xk = xr.next()
            if lat:
                r0 = b * L + blk * 128
                S.dma('sp', xt[:], k.x[r0:r0 + 128, :], writes=[xk])
                mi = b
            else:
                r0 = b * CTX + blk * 128
                S.dma('sp', xt[:], k.ctx[r0:r0 + 128, :], writes=[xk])
                mi = 2
            yield
            sq, sqk = sqr.next()
            S.op('act', lambda e: e.activation(junk[:], xt[:], AF.Square, accum_out=sq[:, 0:1]), reads=[xk], writes=["A.junk", sqk + "a"])
            yield
            S.op('act', lambda e: e.activation(sq[:, 1:2], sq[:, 0:1], AF.Sqrt, scale=1.0 / D, bias=EPS), reads=[sqk + "a"], writes=[sqk + "b"])
            yield
            S.op('dve', lambda e: e.reciprocal(sq[:, 2:3], sq[:, 1:2]), reads=[sqk + "b"], writes=[sqk])
            S.op('dve', lambda e: e.scalar_tensor_tensor(xt[:], xt[:], sq[:, 2:3], A1[mi][:], ALU.mult, ALU.mult),
                 reads=[xk, sqk, f"A1_{mi}"], writes=[xk])
            yield
            hb, hbk = hbr.next()
            S.op('pool', lambda e: e.tensor_tensor(hb[:], xt[:], B1[mi][:], ALU.add), reads=[xk, f"B1_{mi}"], writes=[hbk])
            yield

            def trs(e):
                inst = None
                for kk in range(8):
                    inst = e.transpose(psT[:, kk * 128:(kk + 1) * 128], hb[:, kk * 128:(kk + 1) * 128], ident[:])
                return inst
            S.op('pe', trs, reads=[hbk, "identbf"], writes=["ps7"])
            hT, hTk = hTr.next()
            S.op('act', lambda e: e.copy(hT[:].rearrange("p a b -> p (a b)"), psT), reads=["ps7"], writes=[hTk])
            yield
            yield
            hg_chunks = [3, 4, 5, 6, 7] if lat else [4, 5, 6]
            hbanks = [6, 3, 4, 5, 6]
            for ci, cc in enumerate(hg_chunks):
                bk = hbanks[ci]
                mmgroup(S, k.bank(bk), [(hT[:, kk, :], win[:, kk, cc * 512:(cc + 1) * 512]) for kk in range(8)],
                        reads=[hTk, f"win{cc}"], wkeys=[f"ps{bk}"])
                pg, pgk = pgr.next()
                S.op('act', lambda e: e.copy(pg[:], k.bank(bk)), reads=[f"ps{bk}"], writes=[pgk])
                if lat:
                    S.dma('pool', k.Phg[r0:r0 + 128, (cc - 3) * 512:(cc - 2) * 512], pg[:], reads=[pgk], writes=[f"Phg{b}.{blk}.{cc - 3}"])
                else:
                    S.dma('pool', k.Pctx[r0:r0 + 128, (cc - 4) * 512:(cc - 3) * 512], pg[:], reads=[pgk], writes=[f"Pctx{b}.{blk}.{cc - 4}"])
            if not lat:
                return
            yield
            for cc in range(3):
                mmgroup(S, k.bank(cc), [(hT[:, kk, :], win[:, kk, cc * 512:(cc + 1) * 512]) for kk in range(8)],
                        reads=[hTk, f"win{cc}"], wkeys=[f"ps{cc}"])
            pb, pbk = pbr.next()
            for cc in range(3):
                S.op('act', lambda e: e.copy(pb[:, cc * 512:(cc + 1) * 512], k.bank(cc)), reads=[f"ps{cc}"], writes=[pbk + f".{cc}"])
            uc, uck = ucr.next()
            S.op('dve', lambda e: e.tensor_tensor(uc[:], k.psum[:, 0:1536], CW[:, 1, :], ALU.mult),
                 reads=["ps0", "ps1", "ps2", "CW1"] + [pbk + f".{c_}" for c_ in range(3)], writes=[uck])
            yield
            for cc in range(3):
                mmgroup(S, k.bank(3 + cc), [(k.cbf["shl"][:], pb[:, cc * 512:(cc + 1) * 512])],
                        reads=[pbk + f".{cc}", "shlbf"], wkeys=[f"ps{3 + cc}"])
            tt, ttk = ttr.next()
            S.op('dve', lambda e: e.tensor_tensor(tt[:], k.psum[:, 1536:3072], CW[:, 0, :], ALU.mult),
                 reads=["ps3", "ps4", "ps5", "CW0"], writes=[ttk])
            S.op('pool', lambda e: e.tensor_tensor(uc[:], uc[:], tt[:], ALU.add), reads=[uck, ttk], writes=[uck])
            yield
            for cc in range(3):
                mmgroup(S, k.bank(cc), [(k.cbf["shr"][:], pb[:, cc * 512:(cc + 1) * 512])],
                        reads=[pbk + f".{cc}", "shrbf"], wkeys=[f"ps{cc}"])
            tt, ttk = ttr.next()
            S.op('dve', lambda e: e.tensor_tensor(tt[:], k.psum[:, 0:1536], CW[:, 2, :], ALU.mult),
                 reads=["ps0", "ps1", "ps2", "CW2"], writes=[ttk])
            S.op('pool', lambda e: e.tensor_tensor(uc[:], uc[:], tt[:], ALU.add), reads=[uck, ttk], writes=[uck])
            yield
            S.op('pool', lambda e: e.tensor_tensor(uc[:], uc[:], CB[:], ALU.add), reads=[uck, "CB"], writes=[uck])
            S.dma('pool', k.U[r0:r0 + 128, :], uc[:], reads=[uck], writes=[f"U{b}.{blk}"])

        if os.environ.get("A_NBLK"):
            nb_ = int(os.environ["A_NBLK"])
            blocks = blocks[:nb_] + blocks[4:4 + nb_]
        run_window((unit(*bl) for bl in blocks), KW)


def stage_F(k):
    S, nc = k.S, k.nc
    with Phase(k, "F") as ph:
        zT = ph.tile([33, L], F32, "zT")
        S.dma('sp', zT[:], k.zT, writes=["zT"])
        w1 = ph.tile([33, 64], F32, "w1")
        S.dma('sp', w1[:], k.pos_w1, writes=["w1"])
        w2 = ph.tile([64, 64], F32, "w2")
        S.dma('sp', w2[:], k.pos_w2, writes=["w2"])
        w3 = ph.tile([64, 2048], F32, "w3")
        S.dma('sp', w3[:], k.pos_w3, writes=["w3"])
        sm = ph.tile([64, 8], F32, "sm")
        for i, ap in enumerate([k.pos_b1, k.freq1, k.pos_b2, k.freq2]):
            S.dma('sp', sm[:, i:i + 1], ap, writes=[f"sm{i}"])
        S.op('dve', lambda e: e.tensor_tensor(sm[:, 4:5], sm[:, 0:1], sm[:, 1:2], ALU.mult), reads=["sm0", "sm1"], writes=["sm4"])
        S.op('dve', lambda e: e.tensor_tensor(sm[:, 5:6], sm[:, 2:3], sm[:, 3:4], ALU.mult), reads=["sm2", "sm3"], writes=["sm5"])
        winw = ph.tile([128, 16, 512], F32, "winw")
        for q in range(4):
            S.dma('sp', winw[:, q * 4:(q + 1) * 4, :], k.win[:, q * 4:(q + 1) * 4, :], writes=[f"winw{q}"])
        h1T = ph.tile([64, L], F32, "h1T")
        h2T = ph.tile([64, L], F32, "h2T")
        ar = ph.ring(1, [64, 512], F32, "a")
        m1r = ph.ring(1, [64, 512], F32, "m1")
        m2r = ph.ring(1, [64, 512], F32, "m2")

        def layer(lhsT, lk, src, srck, dst, dstk, fri, fbi):
            for cc in range(4):
                bk = cc % 4
                mmgroup(S, k.bank(bk)[0:64, :], [(lhsT, src[:, cc * 512:(cc + 1) * 512])], reads=[lk, srck], wkeys=[f"ps{bk}"])
                a, ak = ar.next()
                S.op('act', lambda e: e.activation(a[:], k.bank(bk)[0:64, :], AF.Identity, bias=sm[:, fbi:fbi + 1], scale=sm[:, fri:fri + 1]),
                     reads=[f"ps{bk}", f"sm{fri}", f"sm{fbi}"], writes=[ak])
                m1, m1k = m1r.next()
                m2, m2k = m2r.next()
                S.op('dve', lambda e: e.tensor_single_scalar(m1[:], a[:], PI, ALU.is_gt), reads=[ak], writes=[m1k])
                S.op('dve', lambda e: e.tensor_single_scalar(m2[:], a[:], -PI, ALU.is_lt), reads=[ak], writes=[m2k])
                S.op('dve', lambda e: e.tensor_tensor(m2[:], m2[:], m1[:], ALU.subtract), reads=[m1k, m2k], writes=[m2k])
                S.op('dve', lambda e: e.scalar_tensor_tensor(a[:], m2[:], 2.0 * PI, a[:], ALU.mult, ALU.add), reads=[m2k, ak], writes=[ak])
                S.op('act', lambda e: e.activation(dst[:, cc * 512:(cc + 1) * 512], a[:], AF.Sin), reads=[ak], writes=[dstk])

        layer(w1[:], "w1", zT, "zT", h1T, "h1T", 1, 4)
        layer(w2[:], "w2", h1T, "h1T", h2T, "h2T", 3, 5)
        hs = ph.tile([128, 16, 1024], BF16, "hs")
        hd = ph.tile([128, 16, 1024], BF16, "hd")
        hw = ph.tile([128, 2048], F32, "hw")
        absw = ph.ring(2, [128, 2048], BF16, "absw")
        onesb = k.cbf["ones"]
        for blk in range(16):
            for cc in range(4):
                mmgroup(S, k.bank(cc), [(h2T[:, blk * 128:(blk + 1) * 128], w3[:, cc * 512:(cc + 1) * 512])],
                        reads=["h2T", "w3"], wkeys=[f"ps{cc}"])
            hwv = hw[:].rearrange("p (a c) -> p a c", a=4)
            S.op('dve', lambda e: e.tensor_tensor(hwv, k.psum[:, 0:2048].rearrange("p (a c) -> p a c", a=4),
                                                  winw[:, blk, :].unsqueeze(1).to_broadcast([128, 4, 512]), ALU.mult),
                 reads=["ps0", "ps1", "ps2", "ps3", f"winw{blk // 4}"], writes=["hw"])
            if blk == 0:
                hw4 = hw[:].rearrange("p (o d c) -> p o d c", o=2, d=2)
                S.op('dve', lambda e: e.memset(hw4[0:1, :, 1, :], 0.0), reads=[], writes=["hw"])
            ab, abk = absw.next()
            S.op('act', lambda e: e.activation(ab[:], hw[:], AF.Abs), reads=["hw"], writes=[abk])

            def nfn(e):
                inst = None
                for cc in range(4):
                    inst = e.matmul(k.bank(4 + cc), onesb[:], ab[:, cc * 512:(cc + 1) * 512], start=(blk == 0), stop=(blk == 15))
                return inst
            S.op('pe', nfn, reads=[abk, "onesbf"], writes=["ps4", "ps5", "ps6", "ps7"])
            hw4 = hw[:].rearrange("p (o d c) -> p o d c", o=2, d=2)
            hsv = hs[:, blk, :].rearrange("p (o c) -> p o c", o=2)
            hdv = hd[:, blk, :].rearrange("p (o c) -> p o c", o=2)
            S.op('dve', lambda e: e.tensor_tensor(hsv, hw4[:, :, 0, :], hw4[:, :, 1, :], ALU.add), reads=["hw"], writes=[f"hs{blk}"])
            S.op('pool', lambda e: e.tensor_tensor(hdv, hw4[:, :, 1, :], hw4[:, :, 0, :], ALU.subtract), reads=["hw"], writes=[f"hd{blk}"])
        rn = ph.tile([128, 1024], F32, "rn")
        ns4 = k.psum[:, 2048:4096].rearrange("p (o d c) -> p o d c", o=2, d=2)
        rnv = rn[:].rearrange("p (o c) -> p o c", o=2)
        nt = ph.tile([128, 1024], F32, "nt")
        ntv = nt[:].rearrange("p (o c) -> p o c", o=2)
        S.op('dve', lambda e: e.tensor_copy(ntv, ns4[:, :, 0, :]), reads=["ps4", "ps5", "ps6", "ps7"], writes=["nt"])
        S.op('dve', lambda e: e.tensor_tensor(ntv, ntv, ns4[:, :, 1, :], ALU.add), reads=["nt", "ps4", "ps5", "ps6", "ps7"], writes=["nt"])
        S.op('dve', lambda e: e.tensor_scalar(nt[:], nt[:], EPS, float(L), ALU.add, ALU.mult), reads=["nt"], writes=["nt"])
        S.op('dve', lambda e: e.reciprocal(rn[:], nt[:]), reads=["nt"], writes=["rn"])
        tr = ph.ring(2, [128, 2, 2048], BF16, "tab")
        ho = ph.ring(2, [128, 1024], F32, "ho")
        for m in range(16):
            tb, tbk = tr.next()
            for ci in range(2):
                S.dma('sp', tb[:, ci, :], k.tabF[ci, m], writes=[tbk + f".{ci}"])
            for ci, src in enumerate([hs, hd]):
                for half in range(2):
                    bk = ci * 2 + half
                    mmgroup(S, k.bank(bk), [(tb[:, ci, kk * 128:(kk + 1) * 128], src[:, kk, half * 512:(half + 1) * 512]) for kk in range(16)],
                            reads=[tbk + f".{ci}"] + [f"{'hs' if ci == 0 else 'hd'}{kk}" for kk in range(16)], wkeys=[f"ps{bk}"])
            for ci in range(2):
                h, hk = ho.next()
                S.op('dve', lambda e: e.tensor_tensor(h[:], k.psum[:, ci * 1024:(ci + 1) * 1024], rn[:], ALU.mult),
                     reads=[f"ps{2 * ci}", f"ps{2 * ci + 1}", "rn"], writes=[hk])
                S.dma('pool', k.Hs[ci, m], h[:], reads=[hk], writes=[f"Hs{m}"])


def stage_H(k):
    S, nc = k.S, k.nc
    with Phase(k, "H") as ph:
        HB = ph.tile([128, 2, 512], F32, "HB")
        for o in range(2):
            S.dma('sp', HB[:, o, :], bc_row(k.hy_bias[o:o + 1, :], 512), writes=[f"HB{o}"])
        hyG = ph.tile([128, 512], F32, "hyG")
        S.dma('sp', hyG[:], bc_row(k.hy_norm_g, 512), writes=["hyG"])
        curbf = ph.tile([128, 16, 512], BF16, "curbf")
        cur32 = ph.tile([128, 16, 512], F32, "cur32")
        W = ph.tile([128, 2, 16, 512], BF16, "W")
        tr = ph.ring(2, [128, 2, 2048], BF16, "tab")
        Hr = ph.ring(2, [128, 2, 512], F32, "Hri")
        ab = ph.ring(2, [128, 2, 512], F32, "ab")
        t1 = ph.tile([128, 512], F32, "t1")
        t2 = ph.tile([128, 512], F32, "t2")
        t3 = ph.tile([128, 512], F32, "t3")
        t4 = ph.tile([128, 512], F32, "t4")
        xgr = ph.ring(2, [128, 512], F32, "xg")
        tb_ = ph.tile([128, 512], F32, "tb")
        lc = ph.tile([128, 512], F32, "lc")
        zz = ph.ring(2, [128, 512], F32, "zz")
        junk = ph.tile([128, 512], BF16, "junk")
        sqr = ph.ring(2, [128, 4], F32, "sq")
        yb = ph.ring(2, [128, 512], BF16, "yb")
        for b in range(NB):
            T0 = b * L
            uv = k.U[T0:T0 + L, 0:512].rearrange("(k p) c -> p k c", p=128)
            for q in range(4):
                S.dma('pool', curbf[:, q * 4:(q + 1) * 4, :], uv[:, q * 4:(q + 1) * 4, :],
                      reads=[f"U{b}.{i}" for i in range(q * 4, q * 4 + 4)], writes=[f"curbf{i}" for i in range(q * 4, q * 4 + 4)])
                S.dma('sp', cur32[:, q * 4:(q + 1) * 4, :], uv[:, q * 4:(q + 1) * 4, :],
                      reads=[f"U{b}.{i}" for i in range(q * 4, q * 4 + 4)], writes=[f"cur32{i}" for i in range(q * 4, q * 4 + 4)])
            for o in range(2):
                for m in range(16):
                    tb, tbk = tr.next()
                    for ci in range(2):
                        S.dma('sp', tb[:, ci, :], k.tabF[ci, m], writes=[tbk + f".{ci}"])
                    h, hk = Hr.next()
                    for ci in range(2):
                        S.dma('sp', h[:, ci, :], k.Hs[ci, m][:, o * 512:(o + 1) * 512], reads=[f"Hs{m}"], writes=[hk + f".{ci}"])
                    for ci in range(2):
                        bk = (m % 2) * 2 + ci
                        mmgroup(S, k.bank(bk), [(tb[:, ci, kk * 128:(kk + 1) * 128], curbf[:, kk, :]) for kk in range(16)],
                                reads=[tbk + f".{ci}"] + [f"curbf{kk}" for kk in range(16)], wkeys=[f"ps{bk}"])
                    a_, ak = ab.next()
                    for ci in range(2):
                        bk = (m % 2) * 2 + ci
                        S.op('act', lambda e: e.copy(a_[:, ci, :], k.bank(bk)), reads=[f"ps{bk}"], writes=[ak + f".{ci}"])
                    S.op('dve', lambda e: e.tensor_tensor(t1[:], a_[:, 0, :], h[:, 0, :], ALU.mult), reads=[ak + ".0", hk + ".0"], writes=["H.t1"])
                    S.op('pool', lambda e: e.tensor_tensor(t2[:], a_[:, 1, :], h[:, 1, :], ALU.mult), reads=[ak + ".1", hk + ".1"], writes=["H.t2"])
                    S.op('dve', lambda e: e.tensor_tensor(W[:, 0, m, :], t1[:], t2[:], ALU.add), reads=["H.t1", "H.t2"], writes=[f"Wr{m}"])
                    S.op('pool', lambda e: e.tensor_tensor(t3[:], a_[:, 1, :], h[:, 0, :], ALU.mult), reads=[ak + ".1", hk + ".0"], writes=["H.t3"])
                    S.op('dve', lambda e: e.tensor_tensor(t4[:], a_[:, 0, :], h[:, 1, :], ALU.mult), reads=[ak + ".0", hk + ".1"], writes=["H.t4"])
                    S.op('pool', lambda e: e.tensor_tensor(W[:, 1, m, :], t3[:], t4[:], ALU.subtract), reads=["H.t3", "H.t4"], writes=[f"Wi{m}"])
                for m in range(16):
                    tb, tbk = tr.next()
                    for ci in range(2):
                        S.dma('sp', tb[:, ci, :], k.tabI[ci, m], writes=[tbk + f".{ci}"])
                    xg, xgk = xgr.next()
                    r0 = T0 + m * 128
                    S.dma('sp', xg[:], k.U[r0:r0 + 128, (o + 1) * 512:(o + 2) * 512], reads=[f"U{b}.{m}"], writes=[xgk])
                    bk = 4 + (m % 2)
                    pairs = [(tb[:, ci, kk * 128:(kk + 1) * 128], W[:, ci, kk, :]) for ci in range(2) for kk in range(16)]
                    mmgroup(S, k.bank(bk), pairs,
                            reads=[tbk + ".0", tbk + ".1"] + [f"Wr{kk}" for kk in range(16)] + [f"Wi{kk}" for kk in range(16)],
                            wkeys=[f"ps{bk}"])
                    S.op('pool', lambda e: e.tensor_tensor(tb_[:], cur32[:, m, :], HB[:, o, :], ALU.mult),
                         reads=[f"cur32{m}", f"HB{o}"], writes=["H.tb"])
                    S.op('dve', lambda e: e.tensor_tensor(lc[:], k.bank(bk), tb_[:], ALU.add), reads=[f"ps{bk}", "H.tb"], writes=["H.lc"])
                    if o == 0:
                        S.op('pool', lambda e: e.tensor_tensor(cur32[:, m, :], lc[:], xg[:], ALU.mult),
                             reads=["H.lc", xgk], writes=[f"cur32{m}"])
                        S.op('act', lambda e: e.copy(curbf[:, m, :], cur32[:, m, :]), reads=[f"cur32{m}"], writes=[f"curbf{m}"])
                    else:
                        z, zk = zz.next()
                        S.op('pool', lambda e: e.tensor_tensor(z[:], lc[:], xg[:], ALU.mult), reads=["H.lc", xgk], writes=[zk])
                        sq, sqk = sqr.next()
                        rstd = rms_rstd(k, z[:], [zk], junk[:], "H.junk", sq, sqk, 512)
                        y, yk = yb.next()
                        S.op('dve', lambda e: e.scalar_tensor_tensor(y[:], z[:], rstd, hyG[:], ALU.mult, ALU.mult),
                             reads=[zk, sqk, "hyG"], writes=[yk])
                        S.dma('pool', k.Ymix[r0:r0 + 128, 0:512], y[:], reads=[yk], writes=[f"Ymix{b}.{m}a"])


def run_window(gens, K):
    active = []
    it = iter(gens)
    more = True
    while True:
        if len(active) < K and more:
            try:
                active.append(next(it))
            except StopIteration:
                more = False
        if not active:
            break
        for g in list(active):
            try:
                next(g)
            except StopIteration:
                active.remove(g)


def stage_G(k):
    S, nc = k.S, k.nc
    KW = int(os.environ.get("G_WIN", "4"))
    with Phase(k, "G") as ph:
        lbt = ph.tile([128, 2, 1024], F32, "lbraw")
        for l_ in range(2):
            S.dma('sp', lbt[:, l_, :], bc_row(k.hg_lb[l_:l_ + 1, :], 1024), writes=[f"lbraw{l_}"])
        lb = ph.tile([128, 1024], F32, "lb")
        oml = ph.tile([128, 1024], F32, "oml")
        S.op('dve', lambda e: e.tensor_tensor(lb[:], lbt[:, 0, :], lbt[:, 1, :], ALU.subtract), reads=["lbraw0", "lbraw1"], writes=["lb"])
        S.op('act', lambda e: e.activation(lb[:], lb[:], AF.Sigmoid), reads=["lb"], writes=["lb"])
        S.op('dve', lambda e: e.tensor_scalar(oml[:], lb[:], -1.0, 1.0, ALU.mult, ALU.add), reads=["lb"], writes=["oml"])
        hgG = ph.tile([128, 4, 128], F32, "hgG")
        for h in range(4):
            S.dma('sp', hgG[:, h, :], bc_row(k.hg_norm_g, 128), writes=[f"hgG{h}"])
        hgGk = [f"hgG{h}" for h in range(4)]
        selt = ph.tile([128, 2, 4], F32, "selt")
        for d in range(2):
            S.dma('sp', selt[:, d, :], k.sel[d], writes=[f"selt{d}"])
        cum = [k.c32["cumf"], k.c32["cumb"]]
        cumk = ["cumf32", "cumb32"]
        mask = [k.c32["maskf"], k.c32["maskb"]]
        maskk = ["maskf32", "maskb32"]
        identb = k.cbf["ident"]
        rowm = k.smallt[:, 258:260]
        Sst = [ph.tile([128, 512], F32, f"S{b}") for b in range(NB)]
        NQ = KW
        qzs = []
        for i in range(NQ):
            pair = []
            for j in range(2):
                t = ph.tile([128, 4, 128], BF16, f"qz{i}{j}")
                S.op('pool', lambda e: e.memset(t[:], 0.0), writes=[f"qz{i}.{j}"])
                pair.append(t)
            qzs.append(pair)
        qzi = [0]
        psi = [0]

        def psnext():
            i = psi[0] % 8
            psi[0] += 1
            return k.bank(i), f"ps{i}"

        def R(shape, dt, name, n=None):
            return ph.ring(n or KW, shape, dt, name)
        zr, kkr, vr, qr, Er, Eir = (R([128, 512], F32, n_) for n_ in ("z", "kk", "v", "q", "E", "Ei"))
        qtr, ktr, vbr, kz0r, kz1r = (R([128, 512], BF16, n_) for n_ in ("qt", "kt", "vb", "kz0", "kz1"))
        ATr, kTr = R([128, 4, 128], BF16, "AT"), R([128, 2, 4, 128], BF16, "kT")
        ecr, dltr, selsbr = R([128, 4, 4], F32, "ec"), R([128, 4, 2], F32, "dlt"), R([128, 4, 4], F32, "selsb")
        Spr = R([128, 512], BF16, "Sp", 2 * KW)
        tmpr, ofr, gr, orr, sqr2 = (R([128, 512], F32, n_) for n_ in ("tmp", "of", "g", "o", "sq"))
        ssr, ybr = R([128, 8], F32, "ss"), R([128, 512], BF16, "yb")
        qscale = 128.0 ** -0.5
        turn = [0, 0]

        def unit(d, lat, blk, b, idx):
            if lat:
                r0 = b * L + blk * 128
                src, srcp, zc, vc = k.Phg, f"Phg{b}.{blk}.", (2 + d) * 512, 512
            else:
                r0 = b * CTX + blk * 128
                src, srcp, zc, vc = k.Pctx, f"Pctx{b}.{blk}.", (1 + d) * 512, 0
            z, zk = zr.next()
            v, vk = vr.next()
            S.dma('sp', z[:], src[r0:r0 + 128, zc:zc + 512], reads=[srcp + str(zc // 512)], writes=[zk])
            S.dma('sp', v[:], src[r0:r0 + 128, vc:vc + 512], reads=[srcp + str(vc // 512)], writes=[vk])
            if lat:
                q, qk = qr.next()
                S.dma('sp', q[:], src[r0:r0 + 128, 0:512], reads=[srcp + "0"], writes=[qk])
                if d == 1:
                    of_, ofk = ofr.next()
                    S.dma('sp', of_[:], k.Of[r0:r0 + 128, :], reads=[f"Of{b}.{blk}"], writes=[ofk])
                    g, gk = gr.next()
                    S.dma('sp', g[:], k.Phg[r0:r0 + 128, 2048:2560], reads=[srcp + "4"], writes=[gk])
            yield
            S.op('act', lambda e: e.activation(z[:], z[:], AF.Sigmoid), reads=[zk], writes=[zk])
            yield
            S.op('dve', lambda e: e.tensor_tensor(z[:], z[:], oml[:, d * 512:(d + 1) * 512], ALU.mult), reads=[zk, "oml"], writes=[zk])
            yield
            S.op('pool', lambda e: e.tensor_tensor(z[:], z[:], lb[:, d * 512:(d + 1) * 512], ALU.add), reads=[zk, "lb"], writes=[zk])
            yield
            kk_, kkk = kkr.next()
            S.op('act', lambda e: e.activation(kk_[:], z[:], AF.Identity, bias=1.0, scale=-1.0), reads=[zk], writes=[kkk])
            S.op('act', lambda e: e.activation(z[:], z[:], AF.Ln), reads=[zk], writes=[zk])
            lf, lfk = z, zk
            vb, vbk = vbr.next()
            S.op('act', lambda e: e.copy(vb[:], v[:]), reads=[vk], writes=[vbk])
            if lat:
                S.op('act', lambda e: e.activation(q[:], q[:], AF.Silu), reads=[qk], writes=[qk])
                if d == 1:
                    S.op('act', lambda e: e.activation(g[:], g[:], AF.Silu), reads=[gk], writes=[gk])
            yield
            cb, cbk = psnext()
            mmgroup(S, cb, [(cum[d][:], lf[:])], reads=[cumk[d], lfk], wkeys=[cbk])
            sb_, sbk = psnext()
            selps = sb_[:, 0:16].rearrange("p (h c) -> p h c", h=4)

            def self_(e):
                inst = None
                for h in range(4):
                    inst = e.matmul(selps[:, h, :], lf[:, h * 128:(h + 1) * 128], selt[:, d, :], start=True, stop=True)
                return inst
            S.op('pe', self_, reads=[lfk, f"selt{d}"], writes=[sbk])
            Ei, Eik = Eir.next()
            S.op('act', lambda e: e.activation(Ei[:], cb, AF.Exp, scale=-1.0), reads=[cbk], writes=[Eik])
            if lat:
                E, Ek = Er.next()
                S.op('act', lambda e: e.activation(E[:], cb, AF.Exp), reads=[cbk], writes=[Ek])
            ssb, ssbk = selsbr.next()
            S.op('act', lambda e: e.copy(ssb[:], selps), reads=[sbk], writes=[ssbk])
            yield
            ec, eck = ecr.next()
            dlt, dltk = dltr.next()
            S.op('act', lambda e: e.activation(ec[:], ssb[:], AF.Exp), reads=[ssbk], writes=[eck])
            spv = ssb[:].rearrange("p h (j t) -> p h j t", j=2)
            S.op('dve', lambda e: e.tensor_tensor(dlt[:], spv[:, :, :, 1], spv[:, :, :, 0], ALU.subtract), reads=[ssbk], writes=[dltk])
            kt, ktk = ktr.next()
            S.op('dve', lambda e: e.tensor_tensor(kt[:], kk_[:], Ei[:], ALU.mult), reads=[kkk, Eik], writes=[ktk])
            if lat:
                qt, qtk = qtr.next()
                S.op('dve', lambda e: e.scalar_tensor_tensor(qt[:], q[:], qscale, E[:], ALU.mult, ALU.mult), reads=[qk, Ek], writes=[qtk])
            yield
            S.op('act', lambda e: e.activation(dlt[:], dlt[:], AF.Exp), reads=[dltk], writes=[dltk])
            kz = []
            for j, rr_ in enumerate([kz0r, kz1r]):
                kzt, kzk = rr_.next()
                if j == 0:
                    S.op('act', lambda e: e.activation(kzt[:], kt[:], AF.Copy, scale=rowm[:, j:j + 1]), reads=[ktk, "small"], writes=[kzk])
                else:
                    S.op('dve', lambda e: e.tensor_single_scalar(kzt[:], kt[:], rowm[:, j:j + 1], ALU.mult), reads=[ktk, "small"], writes=[kzk])
                kz.append((kzt, kzk))
            yield
            if lat:
                qb_, qbk = psnext()
                psq = qb_.bitcast(BF16)

                def trq(e):
                    inst = None
                    for h in range(4):
                        inst = e.transpose(psq[:, h * 128:(h + 1) * 128], qt[:, h * 128:(h + 1) * 128], identb[:])
                    for h in range(4):
                        inst = e.transpose(psq[:, 512 + h * 128:512 + (h + 1) * 128], kt[:, h * 128:(h + 1) * 128], identb[:])
                    return inst
                S.op('pe', trq, reads=[qtk, ktk, "identbf"], writes=[qbk])
                kT, kTk = kTr.next()
                S.op('act', lambda e: e.copy(kT[:].rearrange("p a h t -> p (a h t)"), psq), reads=[qbk], writes=[kTk])
                yield
                qi = qzi[0] % NQ
                qzi[0] += 1
                qz, qzk = qzs[qi], [f"qz{qi}.0", f"qz{qi}.1"]
                S.op('pool', lambda e: e.tensor_copy(qz[0][:, :, 0:64], kT[:, 0, :, 0:64]), reads=[kTk], writes=[qzk[0]])
                S.op('pool', lambda e: e.tensor_copy(qz[1][:, :, 64:128], kT[:, 0, :, 64:128]), reads=[kTk], writes=[qzk[1]])
                ab_, abk = psnext()
                atps = ab_.rearrange("p (h t) -> p h t", h=4)

                def atf(e):
                    inst = None
                    for h in range(4):
                        inst = e.matmul(atps[:, h, :], kT[:, 1, h, :], kT[:, 0, h, :], start=True, stop=True)
                    return inst
                S.op('pe', atf, reads=[kTk], writes=[abk])
                AT, ATk = ATr.next()
                S.op('dve', lambda e: e.tensor_tensor(AT[:], atps, mask[d][:].unsqueeze(1).to_broadcast([128, 4, 128]), ALU.mult),
                     reads=[abk, maskk[d]], writes=[ATk])
                yield
            while turn[b] != idx:
                yield
            Sk, St = f"S{b}", Sst[b]
            ecv = ec[:].rearrange("p h (j t) -> p h j t", j=2)
            S4 = St[:].rearrange("p (h v) -> p h v", h=4)
            Sps = {}
            for jj, j in enumerate([0, 1] if d == 0 else [1, 0]):
                ecj = ecv[:, :, j, 0:1].to_broadcast([128, 4, 128])
                e1j = ecv[:, :, j, 1:2].to_broadcast([128, 4, 128])
                e2j = dlt[:, :, j:j + 1].to_broadcast([128, 4, 128])
                if lat:
                    Sp, Spk = Spr.next()
                    S.op('dve', lambda e: e.tensor_tensor(Sp[:].rearrange("p (h v) -> p h v", h=4), S4, ecj, ALU.mult),
                         reads=[Sk + f".{h}" for h in range(4)] + [eck], writes=[Spk])
                    Sps[j] = (Sp, Spk)
                kzt, kzk = kz[j]
                stps, stk = psnext()

                def stf(e):
                    inst = None
                    for h in range(4):
                        inst = e.matmul(stps[:, h * 128:(h + 1) * 128], kzt[:, h * 128:(h + 1) * 128], vb[:, h * 128:(h + 1) * 128],
                                        start=True, stop=True)
                    return inst
                S.op('pe', stf, reads=[kzk, vbk], writes=[stk])
                tmp, tmpk = tmpr.next()
                S.op('dve', lambda e: e.tensor_tensor(tmp[:].rearrange("p (h v) -> p h v", h=4), stps.rearrange("p (h v) -> p h v", h=4), e2j, ALU.mult),
                     reads=[stk, dltk], writes=[tmpk])
                for h in range(4):
                    hs_ = slice(h * 128, (h + 1) * 128)
                    S.op('dve', lambda e: e.scalar_tensor_tensor(St[:, hs_], St[:, hs_], ecv[:, h, j, 1:2], tmp[:, hs_], ALU.mult, ALU.add),
                         reads=[Sk + f".{h}", eck, tmpk], writes=[Sk + f".{h}"])
                if jj == 1:
                    turn[b] += 1
                yield
            if not lat:
                return
            ops_, opk = psnext()

            def ofn(e):
                inst = None
                for h in range(4):
                    hs_ = slice(h * 128, (h + 1) * 128)
                    e.matmul(ops_[:, hs_], AT[:, h, :], vb[:, hs_], start=True, stop=False)
                    e.matmul(ops_[:, hs_], qz[0][:, h, :], Sps[0][0][:, hs_], start=False, stop=False)
                    inst = e.matmul(ops_[:, hs_], qz[1][:, h, :], Sps[1][0][:, hs_], start=False, stop=True)
                return inst
            S.op('pe', ofn, reads=[ATk, vbk, Sps[0][1], Sps[1][1]] + qzk, writes=[opk])
            if d == 0:
                of_, ofk = ofr.next()
                S.op('act', lambda e: e.copy(of_[:], ops_), reads=[opk], writes=[ofk])
                S.dma('pool', k.Of[r0:r0 + 128, :], of_[:], reads=[ofk], writes=[f"Of{b}.{blk}"])
                return
            o, ok_ = orr.next()
            S.op('dve', lambda e: e.tensor_tensor(o[:], ops_, of_[:], ALU.add), reads=[opk, ofk], writes=[ok_])
            yield
            sq, sqk = sqr2.next()
            S.op('pool', lambda e: e.tensor_tensor(sq[:], o[:], o[:], ALU.mult), reads=[ok_], writes=[sqk])
            yield
            ss, ssk = ssr.next()
            S.op('dve', lambda e: e.tensor_reduce(ss[:, 0:4], sq[:].rearrange("p (h v) -> p h v", h=4), AX.X, ALU.add), reads=[sqk], writes=[ssk + "a"])
            yield
            S.op('act', lambda e: e.activation(ss[:, 0:4], ss[:, 0:4], AF.Sqrt, scale=1.0 / 128, bias=EPS), reads=[ssk + "a"], writes=[ssk + "a"])
            yield
            S.op('dve', lambda e: e.reciprocal(ss[:, 4:8], ss[:, 0:4]), reads=[ssk + "a"], writes=[ssk])
            S.op('dve', lambda e: e.tensor_tensor(o[:].rearrange("p (h v) -> p h v", h=4), o[:].rearrange("p (h v) -> p h v", h=4),
                                                  ss[:, 4:8].unsqueeze(2).to_broadcast([128, 4, 128]), ALU.mult), reads=[ok_, ssk], writes=[ok_])
            yield
            S.op('pool', lambda e: e.tensor_tensor(o[:], o[:], hgG[:].rearrange("p h v -> p (h v)"), ALU.mult), reads=[ok_] + hgGk, writes=[ok_])
            yield
            y, yk = ybr.next()
            S.op('dve', lambda e: e.tensor_tensor(y[:], o[:], g[:], ALU.mult), reads=[ok_, gk], writes=[yk])
            S.dma('pool', k.Ymix[r0:r0 + 128, 512:1024], y[:], reads=[yk], writes=[f"Ymix{b}.{blk}b"])

        for d in range(2):
            for b in range(NB):
                S.op('pool', lambda e: e.memset(Sst[b][:], 0.0), writes=[f"S{b}.{h}" for h in range(4)])
            turn[0] = turn[1] = 0
            order = [(False, i) for i in ([0, 1] if d == 0 else [1, 0])] + [(True, i) for i in (range(16) if d == 0 else range(15, -1, -1))]
            gens = (unit(d, lat, blk, b, idx) for idx, (lat, blk) in enumerate(order) for b in range(NB))
            run_window(gens, KW)


def stage_O(k):
    S, nc = k.S, k.nc
    KW = int(os.environ.get("O_WIN", "3"))
    with Phase(k, "O") as ph:
        wout = ph.tile([128, 8, D], BF16, "wout")
        wv = k.w_out.rearrange("(k p) n -> p k n", p=128)
        for h in range(2):
            S.dma('pool', wout[:, :, h * 512:(h + 1) * 512], wv[:, :, h * 512:(h + 1) * 512], writes=[f"wout{h}"])
        rw = ph.tile([128, 8, NE], F32, "rw")
        S.dma('sp', rw[:], k.router_w.rearrange("(k p) n -> p k n", p=128), writes=["rw"])
        G1 = [load_mod(k, ph, b, 2, f"G1_{b}") for b in range(NB)]
        A2 = [make_scale(k, ph, b, 4, k.norm2_g, f"A2_{b}") for b in range(NB)]
        B2 = [load_mod(k, ph, b, 3, f"B2_{b}") for b in range(NB)]
        ymr = ph.ring(KW, [128, D], BF16, "ym")
        ymTr = ph.ring(KW, [128, 8, 128], BF16, "ymT")
        xr = ph.ring(KW, [128, D], F32, "x")
        ttr = ph.ring(KW, [128, D], F32, "tt")
        xmr = ph.ring(KW, [128, D], F32, "xm")
        junk = ph.tile([128, D], BF16, "junk")
        sqr = ph.ring(KW, [128, 4], F32, "sq")
        h2r = ph.ring(KW, [128, D], F32, "h2")
        h2br = ph.ring(KW, [128, D], BF16, "h2b")
        h2Tr = ph.ring(KW, [128, 8, 128], F32, "h2T")
        smr = ph.ring(KW, [128, 4], F32, "sm")
        er = ph.ring(KW, [128, NE], F32, "e")
        identb = k.cbf["ident"]
        ident32 = k.c32["ident"]
        psT = k.bank(7).bitcast(BF16)
        psR = k.psum[:, 2 * 512:4 * 512]

        def unit(b, blk):
            r0 = b * L + blk * 128
            ym, ymk = ymr.next()
            S.dma('sp', ym[:], k.Ymix[r0:r0 + 128, :], reads=[f"Ymix{b}.{blk}a", f"Ymix{b}.{blk}b"], writes=[ymk])
            xt, xk = xr.next()
            S.dma('sp', xt[:], k.x[r0:r0 + 128, :], writes=[xk])
            yield

            def trs(e):
                inst = None
                for kk in range(8):
                    inst = e.transpose(psT[:, kk * 128:(kk + 1) * 128], ym[:, kk * 128:(kk + 1) * 128], identb[:])
                return inst
            S.op('pe', trs, reads=[ymk, "identbf"], writes=["ps7"])
            ymT, ymTk = ymTr.next()
            S.op('act', lambda e: e.copy(ymT[:].rearrange("p a b -> p (a b)"), psT), reads=["ps7"], writes=[ymTk])
            yield
            for h in range(2):
                mmgroup(S, k.bank(h), [(ymT[:, kk, :], wout[:, kk, h * 512:(h + 1) * 512]) for kk in range(8)],
                        reads=[ymTk, f"wout{h}"], wkeys=[f"ps{h}"])
            tt, ttk = ttr.next()
            S.op('dve', lambda e: e.tensor_tensor(tt[:], k.psum[:, 0:1024], G1[b][:], ALU.mult), reads=["ps0", "ps1", f"G1_{b}"], writes=[ttk])
            yield
            xm, xmk = xmr.next()
            S.op('pool', lambda e: e.tensor_tensor(xm[:], tt[:], xt[:], ALU.add), reads=[ttk, xk], writes=[xmk])
            S.dma('pool', k.Xmid[r0:r0 + 128, :], xm[:], reads=[xmk], writes=[f"Xmid{b}.{blk}"])
            yield
            sq, sqk = sqr.next()
            S.op('act', lambda e: e.activation(junk[:], xm[:], AF.Square, accum_out=sq[:, 0:1]), reads=[xmk], writes=["O.junk", sqk + "a"])
            yield
            S.op('act', lambda e: e.activation(sq[:, 1:2], sq[:, 0:1], AF.Sqrt, scale=1.0 / D, bias=EPS), reads=[sqk + "a"], writes=[sqk + "b"])
            yield
            S.op('dve', lambda e: e.reciprocal(sq[:, 2:3], sq[:, 1:2]), reads=[sqk + "b"], writes=[sqk])
            h2, h2k = h2r.next()
            S.op('dve', lambda e: e.scalar_tensor_tensor(h2[:], xm[:], sq[:, 2:3], A2[b][:], ALU.mult, ALU.mult), reads=[xmk, sqk, f"A2_{b}"], writes=[h2k])
            yield
            S.op('pool', lambda e: e.tensor_tensor(h2[:], h2[:], B2[b][:], ALU.add), reads=[h2k, f"B2_{b}"], writes=[h2k])
            yield
            h2b, h2bk = h2br.next()
            S.op('act', lambda e: e.copy(h2b[:], h2[:]), reads=[h2k], writes=[h2bk])
            S.dma('pool', k.H2[r0:r0 + 128, :], h2b[:], reads=[h2bk], writes=[f"H2{b}.{blk}"])

            def trr(e):
                inst = None
                for kk in range(8):
                    inst = e.transpose(psR[:, kk * 128:(kk + 1) * 128], h2[:, kk * 128:(kk + 1) * 128], ident32[:])
                return inst
            S.op('pe', trr, reads=[h2k, "ident32"], writes=["ps2", "ps3"])
            h2T, h2Tk = h2Tr.next()
            h2Tf = h2T[:].rearrange("p a b -> p (a b)")
            S.op('act', lambda e: e.copy(h2Tf[:, 0:512], psR[:, 0:512]), reads=["ps2"], writes=[h2Tk + ".0"])
            S.op('dve', lambda e: e.tensor_copy(h2Tf[:, 512:1024], psR[:, 512:1024]), reads=["ps3"], writes=[h2Tk + ".1"])
            yield
            lg = k.bank(4)[:, 0:NE]
            mmgroup(S, lg, [(h2T[:, kk, :], rw[:, kk, :]) for kk in range(8)], reads=[h2Tk + ".0", h2Tk + ".1", "rw"], wkeys=["ps4"])
            sm, smk = smr.next()
            S.op('dve', lambda e: e.tensor_reduce(sm[:, 0:1], lg, AX.X, ALU.max), reads=["ps4"], writes=[smk + "a"])
            S.op('dve', lambda e: e.tensor_single_scalar(sm[:, 1:2], sm[:, 0:1], -1.0, ALU.mult), reads=[smk + "a"], writes=[smk + "b"])
            ee, eek = er.next()
            S.op('act', lambda e: e.activation(ee[:], lg, AF.Exp, bias=sm[:, 1:2], scale=1.0, accum_out=sm[:, 2:3]),
                 reads=["ps4", smk + "b"], writes=[eek, smk + "c"])
            yield
            S.op('dve', lambda e: e.reciprocal(sm[:, 3:4], sm[:, 2:3]), reads=[smk + "c"], writes=[smk + "d"])
            S.op('dve', lambda e: e.tensor_single_scalar(k.aff[:, b, blk, :], ee[:], sm[:, 3:4], ALU.mult), reads=[eek, smk + "d"], writes=[f"aff{b}.{blk}"])

        run_window((unit(b, blk) for b in range(NB) for blk in range(16)), KW)
        if k.debug:
            S.dma('pool', k.dbg_aff, k.aff[:].rearrange("p b k e -> p (b k e)"), reads=[f"aff{b}.{blk}" for b in range(NB) for blk in range(16)], writes=["dbg_aff"])


def stage_R(k):
    S, nc = k.S, k.nc
    affk = [f"aff{b}.{blk}" for b in range(NB) for blk in range(16)]
    with Phase(k, "R") as ph:
        lo = ph.tile([128, NB, NE], F32, "lo")
        hi = ph.tile([128, NB, NE], F32, "hi")
        mid = ph.tile([128, NB, NE], F32, "mid")
        cmp_ = ph.tile([128, NB, 16, NE], F32, "cmp")
        cp = ph.tile([128, NB, NE], F32, "cp")
        ge = ph.tile([128, NB, NE], F32, "ge")
        d1 = ph.tile([128, NB, NE], F32, "d1")
        d2 = ph.tile([128, NB, NE], F32, "d2")
        ones32 = k.c32["ones"]
        S.op('dve', lambda e: e.memset(lo[:], 0.0), writes=["lo"])
        S.op('dve', lambda e: e.memset(hi[:], 1.0), writes=["hi"])
        cps = k.bank(0)[:, 0:NB * NE]
        f2 = lambda t: t[:].rearrange("p b e -> p (b e)")
        for it in range(26):
            S.op('dve', lambda e: e.tensor_tensor(mid[:], lo[:], hi[:], ALU.add), reads=["lo", "hi"], writes=["mid"])
            S.op('dve', lambda e: e.tensor_single_scalar(mid[:], mid[:], 0.5, ALU.mult), reads=["mid"], writes=["mid"])
            S.op('dve', lambda e: e.tensor_tensor(cmp_[:], k.aff[:], mid[:].unsqueeze(2).to_broadcast([128, NB, 16, NE]), ALU.is_gt),
                 reads=affk + ["mid"], writes=["cmp"])
            S.op('dve', lambda e: e.tensor_reduce(cp[:], cmp_[:].rearrange("p b k e -> p b e k"), AX.X, ALU.add), reads=["cmp"], writes=["cp"])
            mmgroup(S, cps, [(ones32[:], f2(cp))], reads=["cp", "ones32"], wkeys=["ps0"])
            S.op('dve', lambda e: e.tensor_single_scalar(f2(ge), cps, float(CAP) - 0.5, ALU.is_gt), reads=["ps0"], writes=["ge"])
            S.op('dve', lambda e: e.tensor_tensor(d1[:], mid[:], lo[:], ALU.subtract), reads=["mid", "lo"], writes=["d1"])
            S.op('dve', lambda e: e.tensor_tensor(d1[:], d1[:], ge[:], ALU.mult), reads=["d1", "ge"], writes=["d1"])
            S.op('dve', lambda e: e.tensor_tensor(d2[:], hi[:], mid[:], ALU.subtract), reads=["hi", "mid"], writes=["d2"])
            S.op('dve', lambda e: e.tensor_tensor(d2[:], d2[:], ge[:], ALU.mult), reads=["d2", "ge"], writes=["d2"])
            S.op('dve', lambda e: e.tensor_tensor(lo[:], lo[:], d1[:], ALU.add), reads=["lo", "d1"], writes=["lo"])
            S.op('dve', lambda e: e.tensor_tensor(hi[:], mid[:], d2[:], ALU.add), reads=["mid", "d2"], writes=["hi"])
        maskb = ph.tile([128, NB, 16, NE], BF16, "maskb")
        S.op('dve', lambda e: e.tensor_tensor(cmp_[:], k.aff[:], lo[:].unsqueeze(2).to_broadcast([128, NB, 16, NE]), ALU.is_gt),
             reads=affk + ["lo"], writes=["cmp"])
        S.op('act', lambda e: e.copy(maskb[:], cmp_[:]), reads=["cmp"], writes=["maskb"])
        pre = k.bank(1).rearrange("p (k b e) -> p k b e", k=16, b=NB)
        onesb, trib = k.cbf["ones"], k.cbf["tri"]

        def pf(e):
            inst = None
            for blk in range(16):
                for bb in range(NB):
                    for pb in range(blk + 1):
                        l = trib if pb == blk else onesb
                        inst = e.matmul(pre[:, blk, bb, :], l[:], maskb[:, bb, pb, :], start=(pb == 0), stop=(pb == blk))
            return inst
        S.op('pe', pf, reads=["maskb", "onesbf", "tribf"], writes=["ps1"])
        v1 = ph.tile([128, NB, 16, NE], F32, "v1")
        for bb in range(NB):
            S.op('dve', lambda e: e.scalar_tensor_tensor(v1[:, bb], pre[:, :, bb, :], 1.0, cmp_[:, bb], ALU.add, ALU.mult),
                 reads=["ps1", "cmp"], writes=["v1"])
        S.op('dve', lambda e: e.tensor_single_scalar(k.val[:], v1[:], -1.0, ALU.add), reads=["v1"], writes=["val"])
        ahi = ph.tile([128, NB, 16, NE], BF16, "ahi")
        a32 = ph.tile([128, NB, 16, NE], F32, "a32")
        S.op('act', lambda e: e.copy(ahi[:], k.aff[:]), reads=affk, writes=["ahi"])
        S.op('dve', lambda e: e.tensor_copy(k.affhl[:, :, :, :, 0], ahi[:]), reads=["ahi"], writes=["affhl0"])
        S.op('dve', lambda e: e.tensor_tensor(a32[:], k.aff[:], ahi[:], ALU.subtract), reads=affk + ["ahi"], writes=["a32"])
        S.op('dve', lambda e: e.tensor_copy(k.affhl[:, :, :, :, 1], a32[:]), reads=["a32"], writes=["affhl1"])
        if k.debug:
            S.dma('pool', k.dbg_val, k.val[:].rearrange("p b k e -> p (b k e)"), reads=["val"], writes=["dbg_val"])


def stage_XG(k):
    S, nc = k.S, k.nc
    with Phase(k, "XG") as ph:
        h2 = ph.tile([128, 16, D], BF16, "h2")
        Pr = ph.ring(2, [128, 16, 2, CAP], BF16, "P")
        xgr = ph.ring(2, [128, 2, 8, CAP], BF16, "xg")
        gtr = ph.ring(2, [128, 4, 2], F32, "gt")
        iota = k.smallt[:, 0:256]
        for b in range(NB):
            hv = k.H2[b * L:(b + 1) * L, :].rearrange("(k p) c -> p k c", p=128)
            for q in range(4):
                S.dma('sp', h2[:, q * 4:(q + 1) * 4, :], hv[:, q * 4:(q + 1) * 4, :],
                      reads=[f"H2{b}.{i}" for i in range(q * 4, q * 4 + 4)], writes=[f"h2_{i}" for i in range(q * 4, q * 4 + 4)])
            h2k = [f"h2_{i}" for i in range(16)]
            for ep in range(NE // 2):
                P_, Pk = Pr.next()
                for e2 in range(2):
                    S.op('dve', lambda e: e.tensor_tensor(P_[:, :, e2, :], iota.unsqueeze(1).to_broadcast([128, 16, CAP]),
                                                          k.val[:, b, :, 2 * ep + e2].unsqueeze(2).to_broadcast([128, 16, CAP]), ALU.is_equal),
                         reads=["val", "small"], writes=[Pk + f".{e2}"])
                xg, xgk = xgr.next()
                for db in range(8):
                    bk = db % 4
                    mmgroup(S, k.bank(bk), [(h2[:, blk, db * 128:(db + 1) * 128], P_[:, blk, :, :].rearrange("p a c -> p (a c)")) for blk in range(16)],
                            reads=h2k + [Pk + ".0", Pk + ".1"], wkeys=[f"ps{bk}"])
                    eng = 'act' if db % 2 == 0 else 'dve'
                    if eng == 'act':
                        S.op('act', lambda e: e.copy(xg[:, :, db, :], k.bank(bk).rearrange("p (a c) -> p a c", a=2)), reads=[f"ps{bk}"], writes=[xgk + f".{db}"])
                    else:
                        S.op('dve', lambda e: e.tensor_copy(xg[:, :, db, :], k.bank(bk).rearrange("p (a c) -> p a c", a=2)), reads=[f"ps{bk}"], writes=[xgk + f".{db}"])
                for e2 in range(2):
                    S.dma('pool', k.XG[2 * ep + e2][:, :, b * CAP:(b + 1) * CAP], xg[:, e2, :, :],
                          reads=[xgk + f".{db}" for db in range(8)], writes=[f"XG{2 * ep + e2}"])
                gps = k.bank(4 + ep % 2)[:, 0:8].rearrange("p (a c) -> p a c", a=4)

                def gf(e):
                    inst = None
                    for e2 in range(2):
                        for ch in range(2):
                            for blk in range(16):
                                inst = e.matmul(gps[:, e2 * 2 + ch, :], P_[:, blk, e2, ch * 128:(ch + 1) * 128],
                                                k.affhl[:, b, blk, 2 * ep + e2, :], start=(blk == 0), stop=(blk == 15))
                    return inst
                S.op('pe', gf, reads=[Pk + ".0", Pk + ".1", "affhl0", "affhl1"], writes=[f"ps{4 + ep % 2}"])
                gt, gtk = gtr.next()
                S.op('act', lambda e: e.copy(gt[:], gps), reads=[f"ps{4 + ep % 2}"], writes=[gtk])
                for e2 in range(2):
                    S.op('dve', lambda e: e.tensor_tensor(k.gates[:, 2 * ep + e2, b, :], gt[:, e2 * 2:e2 * 2 + 2, 0], gt[:, e2 * 2:e2 * 2 + 2, 1], ALU.add),
                         reads=[gtk], writes=[f"gates{2 * ep + e2}.{b}"])


def stage_FF(k):
    S, nc = k.S, k.nc
    with Phase(k, "FF") as ph:
        xgr = ph.ring(2, [128, 8, NB * CAP], BF16, "xg")
        w13r = ph.ring(3, [128, 2, 8, 512], BF16, "w13")
        w2r = ph.ring(2, [128, 16, D], BF16, "w2")
        hid = ph.tile([128, 16, NB * CAP], BF16, "hid")
        sar = ph.ring(2, [128, 512], F32, "sa")
        ysr = ph.ring(2, [128, D], BF16, "ys")
        for e_ in range(NE):
            xg, xgk = xgr.next()
            S.dma('sp', xg[:], k.XG[e_], reads=[f"XG{e_}"], writes=[xgk])
            w1v = k.exp_w1[e_].rearrange("(k p) n -> p k n", p=128)
            w3v = k.exp_w3[e_].rearrange("(k p) n -> p k n", p=128)
            w2v = k.exp_w2[e_].rearrange("(k p) n -> p k n", p=128)
            w2, w2k = w2r.next()
            for fc in range(4):
                w13, w13k = w13r.next()
                S.dma('pool', w13[:, 0, :, :], w1v[:, :, fc * 512:(fc + 1) * 512], writes=[w13k + ".0"])
                S.dma('pool', w13[:, 1, :, :], w3v[:, :, fc * 512:(fc + 1) * 512], writes=[w13k + ".1"])
                S.dma('pool', w2[:, fc * 4:(fc + 1) * 4, :], w2v[:, fc * 4:(fc + 1) * 4, :], writes=[w2k + f".{fc}"])
                for fi in range(4):
                    fidx = fc * 4 + fi
                    ba = (fidx % 2) * 2
                    for mi in range(2):
                        mmgroup(S, k.bank(ba + mi), [(w13[:, mi, kk, fi * 128:(fi + 1) * 128], xg[:, kk, :]) for kk in range(8)],
                                reads=[w13k + f".{mi}", xgk], wkeys=[f"ps{ba + mi}"])
                    sa, sak = sar.next()
                    S.op('act', lambda e: e.activation(sa[:], k.bank(ba), AF.Silu), reads=[f"ps{ba}"], writes=[sak])
                    S.op('dve', lambda e: e.tensor_tensor(hid[:, fidx, :], sa[:], k.bank(ba + 1), ALU.mult), reads=[sak, f"ps{ba + 1}"], writes=[f"hid{fidx}"])
            hidk = [f"hid{i}" for i in range(16)]
            for sc in range(4):
                b, ch = sc // 2, sc % 2
                ys, ysk = ysr.next()
                for dc in range(2):
                    bk = 4 + (sc * 2 + dc) % 4
                    mmgroup(S, k.bank(bk), [(hid[:, f, sc * 128:(sc + 1) * 128], w2[:, f, dc * 512:(dc + 1) * 512]) for f in range(16)],
                            reads=hidk + [w2k + f".{i}" for i in range(4)], wkeys=[f"ps{bk}"])
                    if dc == 0:
                        S.op('act', lambda e: e.activation(ys[:, 0:512], k.bank(bk), AF.Copy, scale=k.gates[:, e_, b, ch:ch + 1]),
                             reads=[f"ps{bk}", f"gates{e_}.{b}"], writes=[ysk + ".0"])
                    else:
                        S.op('dve', lambda e: e.tensor_single_scalar(ys[:, 512:1024], k.bank(bk), k.gates[:, e_, b, ch:ch + 1], ALU.mult),
                             reads=[f"ps{bk}", f"gates{e_}.{b}"], writes=[ysk + ".1"])
                S.dma('sp', k.Y[b][:, e_ * 2 + ch, :], ys[:], reads=[ysk + ".0", ysk + ".1"], writes=[f"Y{b}"])


def stage_SC(k):
    S, nc = k.S, k.nc
    KW = int(os.environ.get("SC_WIN", "3"))
    with Phase(k, "SC") as ph:
        Yb = ph.tile([128, 2 * NE, D], BF16, "Yb")
        G2 = [load_mod(k, ph, b, 5, f"G2_{b}") for b in range(NB)]
        fg = ph.tile([128, D], F32, "fg")
        S.dma('sp', fg[:], bc_row(k.final_g, D), writes=["fg"])
        Dr = ph.ring(KW, [128, NE, 128], BF16, "D")
        PTr = ph.ring(KW, [128, 2, NE, 128], BF16, "PT")
        xmr = ph.ring(KW, [128, D], F32, "xm")
        ttr = ph.ring(KW, [128, D], F32, "tt")
        junk = ph.tile([128, D], BF16, "junk")
        sqr = ph.ring(KW, [128, 4], F32, "sq")
        outr = ph.ring(KW, [128, D], F32, "out")
        identb, onesb = k.cbf["ident"], k.cbf["ones"]
        iotap = k.smallt[:, 256:258]
        vT = k.psum[:, 0:2048].rearrange("p (e t) -> p e t", e=NE)
        cnt = [0]

        def unit(b, blk, Ybk):
            r0 = b * L + blk * 128
            xm, xmk = xmr.next()
            S.dma('sp', xm[:], k.Xmid[r0:r0 + 128, :], reads=[f"Xmid{b}.{blk}"], writes=[xmk])
            Dt, Dk = Dr.next()
            S.op('dve', lambda e: e.tensor_tensor(Dt[:], identb[:].unsqueeze(1).to_broadcast([128, NE, 128]),
                                                  k.val[:, b, blk, :].unsqueeze(2).to_broadcast([128, NE, 128]), ALU.mult),
                 reads=["val", "identbf"], writes=[Dk])
            yield
            Dflat = Dt[:].rearrange("p e t -> p (e t)")

            def vtf(e):
                inst = None
                for c4 in range(4):
                    inst = e.matmul(k.bank(c4), onesb[:], Dflat[:, c4 * 512:(c4 + 1) * 512], start=True, stop=True)
                return inst
            S.op('pe', vtf, reads=[Dk, "onesbf"], writes=["ps0", "ps1", "ps2", "ps3"])
            PT, PTk = PTr.next()
            for ch in range(2):
                S.op('dve', lambda e: e.tensor_single_scalar(PT[:, ch, :, :], vT, iotap[:, ch:ch + 1], ALU.is_equal),
                     reads=["ps0", "ps1", "ps2", "ps3", "small"], writes=[PTk + f".{ch}"])
            yield
            par = cnt[0] % 2
            cnt[0] += 1
            b0 = 4 + 2 * par
            for dc in range(2):
                bk = b0 + dc
                mmgroup(S, k.bank(bk), [(PT[:, ch, e_, :], Yb[:, e_ * 2 + ch, dc * 512:(dc + 1) * 512]) for e_ in range(NE) for ch in range(2)],
                        reads=Ybk + [PTk + ".0", PTk + ".1"], wkeys=[f"ps{bk}"])
            tt, ttk = ttr.next()
            S.op('dve', lambda e: e.tensor_tensor(tt[:], k.psum[:, b0 * 512:(b0 + 2) * 512], G2[b][:], ALU.mult),
                 reads=[f"ps{b0}", f"ps{b0 + 1}", f"G2_{b}"], writes=[ttk])
            yield
            S.op('pool', lambda e: e.tensor_tensor(tt[:], tt[:], xm[:], ALU.add), reads=[ttk, xmk], writes=[ttk])
            yield
            sq, sqk = sqr.next()
            S.op('act', lambda e: e.activation(junk[:], tt[:], AF.Square, accum_out=sq[:, 0:1]), reads=[ttk], writes=["SC.junk", sqk + "a"])
            yield
            S.op('act', lambda e: e.activation(sq[:, 1:2], sq[:, 0:1], AF.Sqrt, scale=1.0 / D, bias=EPS), reads=[sqk + "a"], writes=[sqk + "b"])
            yield
            S.op('dve', lambda e: e.reciprocal(sq[:, 2:3], sq[:, 1:2]), reads=[sqk + "b"], writes=[sqk])
            ot, otk = outr.next()
            S.op('dve', lambda e: e.scalar_tensor_tensor(ot[:], tt[:], sq[:, 2:3], fg[:], ALU.mult, ALU.mult), reads=[ttk, sqk, "fg"], writes=[otk])
            S.dma('pool', k.out[r0:r0 + 128, :], ot[:], reads=[otk], writes=[f"out{b}.{blk}"])

        for b in range(NB):
            for q in range(8):
                S.dma('sp', Yb[:, q * 4:(q + 1) * 4, :], k.Y[b][:, q * 4:(q + 1) * 4, :], reads=[f"Y{b}"], writes=[f"Yb{q}"])
            Ybk = [f"Yb{q}" for q in range(8)]
            run_window((unit(b, blk, Ybk) for blk in range(16)), KW)


_NC_CACHE = {}


def make_in_maps(inputs):
    c = make_consts()
    f32 = lambda a: np.ascontiguousarray(np.asarray(a, dtype=np.float32))
    x = f32(inputs["x"])
    cc = f32(inputs["c"])
    ctx = f32(inputs["ctx"])
    c_ctx = f32(inputs["c_ctx"])
    shared = {
        "ada_w": f32(inputs["ada_w"])[0], "ada_b": f32(inputs["ada_b"]).reshape(1, 6 * D),
        "norm1_g": f32(inputs["norm1_g"]).reshape(1, D), "w_in": f32(inputs["w_in"])[0],
        "hy_conv_w": f32(inputs["hy_conv_w"])[0], "hy_conv_b": f32(inputs["hy_conv_b"]).reshape(1, 1536),
        "hy_pos_w1": f32(inputs["hy_pos_w1"])[0], "hy_pos_b1": f32(inputs["hy_pos_b1"]).reshape(64, 1),
        "hy_freq1": f32(inputs["hy_freq1"]).reshape(64, 1), "hy_pos_w2": f32(inputs["hy_pos_w2"])[0],
        "hy_pos_b2": f32(inputs["hy_pos_b2"]).reshape(64, 1), "hy_freq2": f32(inputs["hy_freq2"]).reshape(64, 1),
        "hy_pos_w3": f32(inputs["hy_pos_w3"])[0], "hy_bias": f32(inputs["hy_bias"])[0],
        "hy_norm_g": f32(inputs["hy_norm_g"]).reshape(1, 512), "hg_lb": f32(inputs["hg_lb"]).reshape(2, 1024),
        "hg_norm_g": f32(inputs["hg_norm_g"]).reshape(1, 128), "w_out": f32(inputs["w_out"])[0],
        "norm2_g": f32(inputs["norm2_g"]).reshape(1, D), "router_w": f32(inputs["router_w"])[0],
        "exp_w1": f32(inputs["exp_w1"])[0], "exp_w3": f32(inputs["exp_w3"])[0], "exp_w2": f32(inputs["exp_w2"])[0],
        "final_g": f32(inputs["final_g"]).reshape(1, D),
        "tabF": c["tabF"].reshape(2, 16, 128, 2048), "tabI": c["tabI"].reshape(2, 16, 128, 2048),
        "zT": c["zT"], "win": c["win"], "m128": c["m128"], "sel": c["sel"], "small": c["small"],
    }
    maps = []
    for i in range(NCORES):
        cl = np.stack([cc[NB * i], cc[NB * i + 1], c_ctx], axis=-1)
        clay = np.ascontiguousarray(cl.reshape(8, 128, 3).transpose(1, 0, 2))
        m = dict(shared)
        m["x"] = np.ascontiguousarray(x[NB * i:NB * i + NB].reshape(NB * L, D))
        m["ctx"] = np.ascontiguousarray(ctx[NB * i:NB * i + NB].reshape(NB * CTX, D))
        m["clay"] = clay
        maps.append(m)
    return maps


def kernel(**inputs):
    if "nc" not in _NC_CACHE:
        _NC_CACHE["nc"] = build_nc()[0]
    nc = _NC_CACHE["nc"]
    maps = make_in_maps(inputs)
    res = run_bass_kernel_spmd(nc, maps, core_ids=list(range(NCORES)))
    outs = [np.asarray(r["out"], dtype=np.float32).reshape(NB, L, D) for r in res.results]
    return np.concatenate(outs, axis=0)
```

```python
import math
import os
from contextlib import ExitStack

import numpy as np
import ml_dtypes

import concourse.bass as bass
import concourse.mybir as mybir
from concourse.bass_utils import run_bass_kernel_spmd

F32 = mybir.dt.float32
BF16 = mybir.dt.bfloat16
ALU = mybir.AluOpType
AF = mybir.ActivationFunctionType
AX = mybir.AxisListType

EPS = 1e-6
NCORES = 8
NB = 2
L = 2048
D = 1024
CTX = 256
NE = 16
CAP = 256
PI = math.pi


class Sched:
    R = 16000
    ND = 16

    def __init__(self, nc, stack):
        self.nc = nc
        self.e = dict(pe=nc.tensor, dve=nc.vector, act=nc.scalar, pool=nc.gpsimd, sp=nc.sync)
        self.csem = {k: [] for k in self.e}
        self.stack = stack
        self.cnt = {k: 0 for k in self.e}
        self.seen = {k: {} for k in self.e}
        self.lastw = {}
        self.readers = {}
        self.dsem = {q: [stack.enter_context(nc.semaphore(f"d_{q}_{i}")) for i in range(self.ND)]
                     for q in ('sp', 'pool')}
        self.duse = {q: [0] * self.ND for q in self.dsem}
        self.dnext = {q: 0 for q in self.dsem}
        self.nwait = 0
        self.ninst = 0

    def _csem(self, eng, idx):
        j = idx // self.R
        while len(self.csem[eng]) <= j:
            self.csem[eng].append(self.stack.enter_context(
                self.nc.semaphore(f"c_{eng}_{len(self.csem[eng])}")))
        return self.csem[eng][j], idx % self.R + 1

    def _deps(self, reads, writes):
        toks = []
        for k in reads:
            w = self.lastw.get(k)
            if w is not None:
                toks.append(w)
        for k in writes:
            w = self.lastw.get(k)
            if w is not None:
                toks.append(w)
            r = self.readers.get(k)
            if r:
                toks.extend(r.values())
        return toks

    def _wait(self, eng, toks):
        best = {}
        for (teng, sem, val) in toks:
            if teng == 'pe' and eng == 'pe':
                continue
            sid = id(sem)
            if val > best.get(sid, (None, 0))[1]:
                best[sid] = (sem, val)
        seen = self.seen[eng]
        for sid, (sem, val) in best.items():
            if seen.get(sid, 0) >= val:
                continue
            self.e[eng].wait_ge(sem, val)
            self.nwait += 1
            seen[sid] = val

    def _reg(self, tok, reads, writes):
        sid = id(tok[1])
        for k in reads:
            self.readers.setdefault(k, {})[sid] = tok
        for k in writes:
            self.lastw[k] = tok
            self.readers[k] = {}

    def op(self, eng, fn, reads=(), writes=()):
        self._wait(eng, self._deps(reads, writes))
        inst = fn(self.e[eng])
        idx = self.cnt[eng]
        self.cnt[eng] += 1
        sem, val = self._csem(eng, idx)
        inst.then_inc(sem, 1)
        self.ninst += 1
        tok = (eng, sem, val)
        self._reg(tok, reads, writes)
        return tok

    def dma(self, q, out, in_, reads=(), writes=(), **kw):
        toks = self._deps(reads, writes)
        j = self.dnext[q]
        self.dnext[q] = (j + 1) % self.ND
        sem = self.dsem[q][j]
        uses = self.duse[q][j]
        if uses > 0:
            toks.append(('dma', sem, 16 * uses))
        self._wait(q, toks)
        inst = self.e[q].dma_start(out=out, in_=in_, **kw)
        inst.then_inc(sem, 16)
        self.ninst += 1
        self.duse[q][j] = uses + 1
        tok = ('dma', sem, 16 * (uses + 1))
        self._reg(tok, reads, writes)
        return tok

    def barrier(self):
        toks = []
        for eng in self.e:
            if self.cnt[eng] > 0:
                sem, val = self._csem(eng, self.cnt[eng] - 1)
                toks.append((eng + '_b', sem, val))
        for q in self.dsem:
            for j in range(self.ND):
                if self.duse[q][j] > 0:
                    toks.append(('dma', self.dsem[q][j], 16 * self.duse[q][j]))
        for eng in self.e:
            self._wait(eng, toks)


class K:
    pass


class Phase:
    def __init__(self, k, name):
        self.k = k
        self.name = name
        self.st = ExitStack()
        self.n = 0

    def __enter__(self):
        self.st.__enter__()
        return self

    def __exit__(self, *a):
        self.k.S.barrier()
        return self.st.__exit__(*a)

    def tile(self, shape, dt, name=None):
        self.n += 1
        nm = f"{self.name}_{name or 't'}_{self.n}"
        return self.st.enter_context(self.k.nc.sbuf_tensor(nm, list(shape), dt))

    def ring(self, n, shape, dt, name):
        return Ring([(self.tile(shape, dt, f"{name}{i}"), f"{self.name}.{name}{i}") for i in range(n)])


class Ring:
    def __init__(self, items):
        self.items = items
        self.i = 0

    def next(self):
        it = self.items[self.i % len(self.items)]
        self.i += 1
        return it


def mmgroup(S, out, pairs, reads, wkeys):
    def fn(e):
        n = len(pairs)
        inst = None
        for i, (l, r) in enumerate(pairs):
            inst = e.matmul(out, l, r, start=(i == 0), stop=(i == n - 1))
        return inst
    return S.op('pe', fn, reads=reads, writes=wkeys)


def bc_row(ap_row, n, parts=128):
    return ap_row.to_broadcast([parts, n])


_CONST_CACHE = {}


def make_consts():
    if _CONST_CACHE:
        return _CONST_CACHE
    bf = ml_dtypes.bfloat16
    N = 2 * L
    s = np.arange(L, dtype=np.float64)[:, None]
    f = np.arange(L, dtype=np.float64)[None, :] + 0.5
    ang = (2.0 * np.pi / N) * s * f
    C = np.cos(ang)
    Sn = np.sin(ang)

    def lay(M):
        return np.ascontiguousarray(M.reshape(16, 128, 16, 128).transpose(2, 1, 0, 3))

    c = {}
    c["tabF"] = np.stack([lay(C), lay(Sn)]).astype(bf)
    c["tabI"] = np.stack([lay(C.T), lay(Sn.T)]).astype(bf)
    t = np.linspace(0.0, 1.0, L, dtype=np.float32)[:, None]
    w = (2.0 * np.float32(math.pi) * np.arange(L, dtype=np.float32)[:, None] / np.float32(L)).astype(np.float32)
    fb = np.linspace(1e-4, 15, 16, dtype=np.float32)[None, :]
    z = np.concatenate([t, np.cos(fb * w), -np.sin(fb * w)], axis=-1).astype(np.float32)
    c["zT"] = np.ascontiguousarray(z.T)
    min_decay = math.log(1e-2) / 1.5
    max_decay = math.log(1e-2) / 0.3
    deltas = np.abs(np.linspace(min_decay, max_decay, 512, dtype=np.float32))
    window = (np.exp(-t * deltas[None, :]) + np.float32(0.05)).astype(np.float32)
    c["win"] = np.ascontiguousarray(window.reshape(16, 128, 512).transpose(1, 0, 2))
    idx = np.arange(128)
    ss, tt = idx[:, None], idx[None, :]
    m128 = {}
    m128["ident"] = (ss == tt)
    m128["ones"] = np.ones((128, 128))
    m128["shl"] = (ss == tt - 1) & (tt % 64 != 0)
    m128["shr"] = (ss == tt + 1) & (tt % 64 != 63)
    same = (ss // 64) == (tt // 64)
    anch = (tt // 64) * 64 + 31
    m128["cumf"] = (same & (ss <= tt)).astype(np.float64) - (same & (ss <= anch)).astype(np.float64)
    m128["cumb"] = (same & (ss >= tt)).astype(np.float64) - (same & (ss >= anch)).astype(np.float64)
    m128["maskf"] = same & (ss <= tt)
    m128["maskb"] = same & (ss >= tt)
    m128["tri"] = ss < tt
    names = ["ident", "ones", "shl", "shr", "cumf", "cumb", "maskf", "maskb", "tri"]
    c["m128"] = np.stack([m128[n].astype(np.float32) for n in names])
    c["m128_names"] = names
    sel = np.zeros((2, 128, 4), np.float32)
    for j in range(2):
        inch = (idx // 64) == j
        a = j * 64 + 31
        sel[0, :, 2 * j] = inch & (idx <= a)
        sel[0, :, 2 * j + 1] = inch
        sel[1, :, 2 * j] = inch & (idx >= a)
        sel[1, :, 2 * j + 1] = inch
    c["sel"] = sel
    small = np.zeros((128, 260), np.float32)
    small[:, 0:256] = np.arange(256)[None, :]
    small[:, 256] = idx
    small[:, 257] = idx + 128
    small[:, 258] = (idx < 64)
    small[:, 259] = (idx >= 64)
    c["small"] = small
    _CONST_CACHE.update(c)
    return _CONST_CACHE


def build_nc(debug=False, stop_after=None):
    nc = bass.Bass("TRN2", target_bir_lowering=False)
    k = K()
    k.nc = nc
    k.debug = debug
    k.dbg_out = []

    def din(name, shape, dt=F32):
        return nc.dram_tensor(name, list(shape), dt, kind="ExternalInput").ap()

    def dscr(name, shape, dt=F32):
        kind = "ExternalOutput" if debug else "Internal"
        if debug:
            k.dbg_out.append(name)
        return nc.dram_tensor(name, list(shape), dt, kind=kind).ap()

    k.x = din("x", [NB * L, D])
    k.ctx = din("ctx", [NB * CTX, D])
    k.clay = din("clay", [128, 8, 3])
    k.ada_w = din("ada_w", [D, 6 * D])
    k.ada_b = din("ada_b", [1, 6 * D])
    k.norm1_g = din("norm1_g", [1, D])
    k.w_in = din("w_in", [D, 4096])
    k.hy_conv_w = din("hy_conv_w", [3, 1536])
    k.hy_conv_b = din("hy_conv_b", [1, 1536])
    k.pos_w1 = din("hy_pos_w1", [33, 64])
    k.pos_b1 = din("hy_pos_b1", [64, 1])
    k.freq1 = din("hy_freq1", [64, 1])
    k.pos_w2 = din("hy_pos_w2", [64, 64])
    k.pos_b2 = din("hy_pos_b2", [64, 1])
    k.freq2 = din("hy_freq2", [64, 1])
    k.pos_w3 = din("hy_pos_w3", [64, 2048])
    k.hy_bias = din("hy_bias", [2, 512])
    k.hy_norm_g = din("hy_norm_g", [1, 512])
    k.hg_lb = din("hg_lb", [2, 1024])
    k.hg_norm_g = din("hg_norm_g", [1, 128])
    k.w_out = din("w_out", [D, D])
    k.norm2_g = din("norm2_g", [1, D])
    k.router_w = din("router_w", [D, NE])
    k.exp_w1 = din("exp_w1", [NE, D, 2 * D])
    k.exp_w3 = din("exp_w3", [NE, D, 2 * D])
    k.exp_w2 = din("exp_w2", [NE, 2 * D, D])
    k.final_g = din("final_g", [1, D])
    k.tabF = din("tabF", [2, 16, 128, 16 * 128], BF16)
    k.tabI = din("tabI", [2, 16, 128, 16 * 128], BF16)
    k.zT = din("zT", [33, L])
    k.win = din("win", [128, 16, 512])
    k.m128 = din("m128", [9, 128, 128])
    k.sel = din("sel", [2, 128, 4])
    k.small = din("small", [128, 260])
    k.out = nc.dram_tensor("out", [NB * L, D], F32, kind="ExternalOutput").ap()
    k.modv = dscr("modv", [3, 6 * D])
    k.U = dscr("U", [NB * L, 1536])
    k.Phg = dscr("Phg", [NB * L, 2560])
    k.Pctx = dscr("Pctx", [NB * CTX, 1536])
    k.Hs = dscr("Hs", [2, 16, 128, 1024])
    k.Ymix = dscr("Ymix", [NB * L, D], BF16)
    k.Of = dscr("Of", [NB * L, 512])
    k.Xmid = dscr("Xmid", [NB * L, D])
    k.H2 = dscr("H2", [NB * L, D], BF16)
    k.XG = dscr("XG", [NE, 128, 8, NB * CAP], BF16)
    k.Y = dscr("Y", [NB, 128, 2 * NE, D], BF16)
    if debug:
        k.dbg_aff = dscr("dbg_aff", [128, NB * 16 * NE])
        k.dbg_val = dscr("dbg_val", [128, NB * 16 * NE])

    stages = ["P", "F", "A", "H", "G", "O", "R", "XG", "FF", "SC"]
    if stop_after is not None:
        stages = stages[:stages.index(stop_after) + 1]
    import os
    if os.environ.get("K_STAGES"):
        stages = os.environ["K_STAGES"].split(",")

    with ExitStack() as st:
        S = Sched(nc, st)
        k.S = S
        k.psum = st.enter_context(nc.psum_tensor("ps", [128, 4096], F32))
        k.bank = lambda i: k.psum[:, i * 512:(i + 1) * 512]
        with Phase(k, "G0") as g0:
            mnames = make_consts()["m128_names"]
            k.c32 = {}
            k.cbf = {}
            for i, n in enumerate(mnames):
                t32 = g0.tile([128, 128], F32, n + "32")
                S.dma('sp', t32[:], k.m128[i], writes=[n + "32"])
                k.c32[n] = t32
                tb = g0.tile([128, 128], BF16, n + "bf")
                S.dma('pool', tb[:], k.m128[i], writes=[n + "bf"])
                k.cbf[n] = tb
            k.smallt = g0.tile([128, 260], F32, "small")
            S.dma('sp', k.smallt[:], k.small, writes=["small"])
            k.aff = g0.tile([128, NB, 16, NE], F32, "aff")
            k.val = g0.tile([128, NB, 16, NE], F32, "val")
            k.affhl = g0.tile([128, NB, 16, NE, 2], BF16, "affhl")
            k.gates = g0.tile([128, NE, NB, 2], F32, "gates")
            S.barrier()
            fns = dict(P=stage_P, F=stage_F, A=stage_A, H=stage_H, G=stage_G, O=stage_O, R=stage_R,
                       XG=stage_XG, FF=stage_FF, SC=stage_SC)
            for sname in stages:
                fns[sname](k)
                S.barrier()
        S.barrier()
    k.stats = (S.ninst, S.nwait)
    return nc, k


def stage_P(k):
    S, nc = k.S, k.nc
    with Phase(k, "P") as ph:
        sT = ph.tile([128, 8, 3], F32, "sT")
        sTs = ph.tile([128, 8, 3], F32, "sTs")
        adab = ph.tile([3, 6 * D], F32, "adab")
        S.dma('sp', sT[:], k.clay, writes=["sT"])
        S.dma('sp', adab[:], bc_row(k.ada_b, 6 * D, 3), writes=["adab"])
        S.op('act', lambda e: e.activation(sTs[:], sT[:], AF.Silu), reads=["sT"], writes=["sTs"])
        wr = ph.ring(2, [128, 8, 512], F32, "w")
        rr = ph.ring(2, [3, 512], F32, "r")
        awv = k.ada_w.rearrange("(k p) n -> p k n", p=128)
        for cc in range(12):
            wt, wk = wr.next()
            S.dma('sp', wt[:], awv[:, :, cc * 512:(cc + 1) * 512], writes=[wk])
            b = cc % 2
            mmgroup(S, k.bank(b)[0:3, :], [(sTs[:, kk, :], wt[:, kk, :]) for kk in range(8)],
                    reads=["sTs", wk], wkeys=[f"ps{b}"])
            r, rk = rr.next()
            S.op('dve', lambda e: e.tensor_tensor(r[:], k.bank(b)[0:3, :], adab[:, cc * 512:(cc + 1) * 512], ALU.add),
                 reads=[f"ps{b}", "adab"], writes=[rk])
            S.dma('pool', k.modv[:, cc * 512:(cc + 1) * 512], r[:], reads=[rk], writes=["modv"])


def load_mod(k, ph, b, which, name):
    t = ph.tile([128, D], F32, name)
    k.S.dma('sp', t[:], bc_row(k.modv[b:b + 1, which * D:(which + 1) * D], D), reads=["modv"], writes=[name])
    return t


def make_scale(k, ph, b, which_sc, gain_ap, name):
    S = k.S
    sc = load_mod(k, ph, b, which_sc, name + "_sc")
    g = ph.tile([128, D], F32, name + "_g")
    S.dma('sp', g[:], bc_row(gain_ap, D), writes=[name + "_g"])
    a = ph.tile([128, D], F32, name)
    S.op('dve', lambda e: e.scalar_tensor_tensor(a[:], sc[:], 1.0, g[:], ALU.add, ALU.mult),
         reads=[name + "_sc", name + "_g"], writes=[name])
    return a


def rms_rstd(k, src, src_keys, junk, junk_key, sq, sqk, n, ncols=1):
    S = k.S
    S.op('act', lambda e: e.activation(junk, src, AF.Square, accum_out=sq[:, 0:1]),
         reads=src_keys, writes=[junk_key, sqk + "a"])
    S.op('act', lambda e: e.activation(sq[:, 1:2], sq[:, 0:1], AF.Sqrt, scale=1.0 / n, bias=EPS),
         reads=[sqk + "a"], writes=[sqk + "b"])
    S.op('dve', lambda e: e.reciprocal(sq[:, 2:3], sq[:, 1:2]), reads=[sqk + "b"], writes=[sqk])
    return sq[:, 2:3]


def stage_A(k):
    S, nc = k.S, k.nc
    KW = int(os.environ.get("A_WIN", "5"))
    with Phase(k, "A") as ph:
        win = ph.tile([128, 8, 4096], BF16, "win")
        wv = k.w_in.rearrange("(k p) n -> p k n", p=128)
        for cc in range(8):
            S.dma('pool', win[:, :, cc * 512:(cc + 1) * 512], wv[:, :, cc * 512:(cc + 1) * 512], writes=[f"win{cc}"])
        A1 = [ph.tile([128, D], F32, f"A1_{b}") for b in range(3)]
        B1 = []
        with Phase(k, "Atmp") as pt:
            for b in range(3):
                a_ = make_scale(k, pt, b, 1, k.norm1_g, f"A1t_{b}")
                S.op('pool', lambda e: e.tensor_copy(A1[b][:], a_[:]), reads=[f"A1t_{b}"], writes=[f"A1_{b}"])
        for b in range(3):
            B1.append(load_mod(k, ph, b, 0, f"B1_{b}"))
        CW = ph.tile([128, 3, 1536], F32, "CW")
        for j in range(3):
            S.dma('sp', CW[:, j, :], bc_row(k.hy_conv_w[j:j + 1, :], 1536), writes=[f"CW{j}"])
        CB = ph.tile([128, 1536], F32, "CB")
        S.dma('sp', CB[:], bc_row(k.hy_conv_b, 1536), writes=["CB"])
        xr = ph.ring(5, [128, D], F32, "x")
        junk = ph.tile([128, D], BF16, "junk")
        sqr = ph.ring(5, [128, 4], F32, "sq")
        hbr = ph.ring(3, [128, D], BF16, "hb")
        hTr = ph.ring(4, [128, 8, 128], BF16, "hT")
        pbr = ph.ring(3, [128, 1536], BF16, "pb")
        ucr = ph.ring(4, [128, 1536], F32, "uc")
        ttr = ph.ring(1, [128, 1536], F32, "tt")
        pgr = ph.ring(2, [128, 512], F32, "pg")
        ident = k.cbf["ident"]
        blocks = [(b, i, False) for b in range(NB) for i in range(2)] + [(b, i, True) for b in range(NB) for i in range(16)]
        psT = k.bank(7).bitcast(BF16)

        def unit(b, blk, lat):
            xt, xk = xr.next()
            if lat:
                r0 = b * L + blk * 128
                S.dma('sp', xt[:], k.x[r0:r0 + 128, :], writes=[xk])
                mi = b
            else:
                r0 = b * CTX + blk * 128
                S.dma('sp', xt[:], k.ctx[r0:r0 + 128, :], writes=[xk])
                mi = 2
            yield
            sq, sqk = sqr.next()
            S.op('act', lambda e: e.activation(junk[:], xt[:], AF.Square, accum_out=sq[:, 0:1]), reads=[xk], writes=["A.junk", sqk + "a"])
            yield
            S.op('act', lambda e: e.activation(sq[:, 1:2], sq[:, 0:1], AF.Sqrt, scale=1.0 / D, bias=EPS), reads=[sqk + "a"], writes=[sqk + "b"])
            yield
            S.op('dve', lambda e: e.reciprocal(sq[:, 2:3], sq[:, 1:2]), reads=[sqk + "b"], writes=[sqk])
            S.op('dve', lambda e: e.scalar_tensor_tensor(xt[:], xt[:], sq[:, 2:3], A1[mi][:], ALU.mult, ALU.mult),
                 reads=[xk, sqk, f"A1_{mi}"], writes=[xk])
            yield
            hb, hbk = hbr.next()
            S.op('pool', lambda e: e.tensor_tensor(hb[:], xt[:], B1[mi][:], ALU.add), reads=[xk, f"B1_{mi}"], writes=[hbk])
            yield

            def trs(e):
                inst = None
                for kk in range(8):
                    inst = e.transpose(psT[:, kk * 128:(kk + 1) * 128], hb[:, kk * 128:(kk + 1) * 128], ident[:])
                return inst
            S.op('pe', trs, reads=[hbk, "identbf"], writes=["ps7"])
            hT, hTk = hTr.next()
            S.op('act', lambda e: e.copy(hT[:].rearrange("p a b -> p (a b)"), psT), reads=["ps7"], writes=[hTk])
            yield
            yield
            hg_chunks = [3, 4, 5, 6, 7] if lat else [4, 5, 6]
            hbanks = [6, 3, 4, 5, 6]
            for ci, cc in enumerate(hg_chunks):
                bk = hbanks[ci]
                mmgroup(S, k.bank(bk), [(hT[:, kk, :], win[:, kk, cc * 512:(cc + 1) * 512]) for kk in range(8)],
                        reads=[hTk, f"win{cc}"], wkeys=[f"ps{bk}"])
                pg, pgk = pgr.next()
                S.op('act', lambda e: e.copy(pg[:], k.bank(bk)), reads=[f"ps{bk}"], writes=[pgk])
                if lat:
                    S.dma('pool', k.Phg[r0:r0 + 128, (cc - 3) * 512:(cc - 2) * 512], pg[:], reads=[pgk], writes=[f"Phg{b}.{blk}.{cc - 3}"])
                else:
                    S.dma('pool', k.Pctx[r0:r0 + 128, (cc - 4) * 512:(cc - 3) * 512], pg[:], reads=[pgk], writes=[f"Pctx{b}.{blk}.{cc - 4}"])
            if not lat:
                return
            yield
            for cc in range(3):
                mmgroup(S, k.bank(cc), [(hT[:, kk, :], win[:, kk, cc * 512:(cc + 1) * 512]) for kk in range(8)],
                        reads=[hTk, f"win{cc}"], wkeys=[f"ps{cc}"])
            pb, pbk = pbr.next()
            for cc in range(3):
                S.op('act', lambda e: e.copy(pb[:, cc * 512:(cc + 1) * 512], k.bank(cc)), reads=[f"ps{cc}"], writes=[pbk + f".{cc}"])
            uc, uck = ucr.next()
            S.op('dve', lambda e: e.tensor_tensor(uc[:], k.psum[:, 0:1536], CW[:, 1, :], ALU.mult),
                 reads=["ps0", "ps1", "ps2", "CW1"] + [pbk + f".{c_}" for c_ in range(3)], writes=[uck])
            yield
            for cc in range(3):
                mmgroup(S, k.bank(3 + cc), [(k.cbf["shl"][:], pb[:, cc * 512:(cc + 1) * 512])],
                        reads=[pbk + f".{cc}", "shlbf"], wkeys=[f"ps{3 + cc}"])
            tt, ttk = ttr.next()
            S.op('dve', lambda e: e.tensor_tensor(tt[:], k.psum[:, 1536:3072], CW[:, 0, :], ALU.mult),
                 reads=["ps3", "ps4", "ps5", "CW0"], writes=[ttk])
            S.op('pool', lambda e: e.tensor_tensor(uc[:], uc[:], tt[:], ALU.add), reads=[uck, ttk], writes=[uck])
            yield
            for cc in range(3):
                mmgroup(S, k.bank(cc), [(k.cbf["shr"][:], pb[:, cc * 512:(cc + 1) * 512])],
                        reads=[pbk + f".{cc}", "shrbf"], wkeys=[f"ps{cc}"])
            tt, ttk = ttr.next()
            S.op('dve', lambda e: e.tensor_tensor(tt[:], k.psum[:, 0:1536], CW[:, 2, :], ALU.mult),
                 reads=["ps0", "ps1", "ps2", "CW2"], writes=[ttk])
            S.op('pool', lambda e: e.tensor_tensor(uc[:], uc[:], tt[:], ALU.add), reads=[uck, ttk], writes=[uck])
            yield
            S.op('dve', lambda e: e.tensor_tensor(uc[:], uc[:], CB[:], ALU.add), reads=[uck, "CB"], writes=[uck])
            S.dma('pool', k.U[r0:r0 + 128, :], uc[:], reads=[uck], writes=[f"U{b}.{blk}"])

        if os.environ.get("A_NBLK"):
            nb_ = int(os.environ["A_NBLK"])
            blocks = blocks[:nb_] + blocks[4:4 + nb_]
        run_window((unit(*bl) for bl in blocks), KW)


def stage_F(k):
    S, nc = k.S, k.nc
    with Phase(k, "F") as ph:
        zT = ph.tile([33, L], F32, "zT")
        S.dma('sp', zT[:], k.zT, writes=["zT"])
        w1 = ph.tile([33, 64], F32, "w1")
        S.dma('sp', w1[:], k.pos_w1, writes=["w1"])
        w2 = ph.tile([64, 64], F32, "w2")
        S.dma('sp', w2[:], k.pos_w2, writes=["w2"])
        w3 = ph.tile([64, 2048], F32, "w3")
        S.dma('sp', w3[:], k.pos_w3, writes=["w3"])
        sm = ph.tile([64, 8], F32, "sm")
        for i, ap in enumerate([k.pos_b1, k.freq1, k.pos_b2, k.freq2]):
            S.dma('sp', sm[:, i:i + 1], ap, writes=[f"sm{i}"])
        S.op('dve', lambda e: e.tensor_tensor(sm[:, 4:5], sm[:, 0:1], sm[:, 1:2], ALU.mult), reads=["sm0", "sm1"], writes=["sm4"])
        S.op('dve', lambda e: e.tensor_tensor(sm[:, 5:6], sm[:, 2:3], sm[:, 3:4], ALU.mult), reads=["sm2", "sm3"], writes=["sm5"])
        winw = ph.tile([128, 16, 512], F32, "winw")
        for q in range(4):
            S.dma('sp', winw[:, q * 4:(q + 1) * 4, :], k.win[:, q * 4:(q + 1) * 4, :], writes=[f"winw{q}"])
        h1T = ph.tile([64, L], F32, "h1T")
        h2T = ph.tile([64, L], F32, "h2T")
        ar = ph.ring(1, [64, 512], F32, "a")
        m1r = ph.ring(1, [64, 512], F32, "m1")
        m2r = ph.ring(1, [64, 512], F32, "m2")

        def layer(lhsT, lk, src, srck, dst, dstk, fri, fbi):
            for cc in range(4):
                bk = cc % 4
                mmgroup(S, k.bank(bk)[0:64, :], [(lhsT, src[:, cc * 512:(cc + 1) * 512])], reads=[lk, srck], wkeys=[f"ps{bk}"])
                a, ak = ar.next()
                S.op('act', lambda e: e.activation(a[:], k.bank(bk)[0:64, :], AF.Identity, bias=sm[:, fbi:fbi + 1], scale=sm[:, fri:fri + 1]),
                     reads=[f"ps{bk}", f"sm{fri}", f"sm{fbi}"], writes=[ak])
                m1, m1k = m1r.next()
                m2, m2k = m2r.next()
                S.op('dve', lambda e: e.tensor_single_scalar(m1[:], a[:], PI, ALU.is_gt), reads=[ak], writes=[m1k])
                S.op('dve', lambda e: e.tensor_single_scalar(m2[:], a[:], -PI, ALU.is_lt), reads=[ak], writes=[m2k])
                S.op('dve', lambda e: e.tensor_tensor(m2[:], m2[:], m1[:], ALU.subtract), reads=[m1k, m2k], writes=[m2k])
                S.op('dve', lambda e: e.scalar_tensor_tensor(a[:], m2[:], 2.0 * PI, a[:], ALU.mult, ALU.add), reads=[m2k, ak], writes=[ak])
                S.op('act', lambda e: e.activation(dst[:, cc * 512:(cc + 1) * 512], a[:], AF.Sin), reads=[ak], writes=[dstk])

        layer(w1[:], "w1", zT, "zT", h1T, "h1T", 1, 4)
        layer(w2[:], "w2", h1T, "h1T", h2T, "h2T", 3, 5)
        hs = ph.tile([128, 16, 1024], BF16, "hs")
        hd = ph.tile([128, 16, 1024], BF16, "hd")
        hw = ph.tile([128, 2048], F32, "hw")
        absw = ph.ring(2, [128, 2048], BF16, "absw")
        onesb = k.cbf["ones"]
        for blk in range(16):
            for cc in range(4):
                mmgroup(S, k.bank(cc), [(h2T[:, blk * 128:(blk + 1) * 128], w3[:, cc * 512:(cc + 1) * 512])],
                        reads=["h2T", "w3"], wkeys=[f"ps{cc}"])
            hwv = hw[:].rearrange("p (a c) -> p a c", a=4)
            S.op('dve', lambda e: e.tensor_tensor(hwv, k.psum[:, 0:2048].rearrange("p (a c) -> p a c", a=4),
                                                  winw[:, blk, :].unsqueeze(1).to_broadcast([128, 4, 512]), ALU.mult),
                 reads=["ps0", "ps1", "ps2", "ps3", f"winw{blk // 4}"], writes=["hw"])
            if blk == 0:
                hw4 = hw[:].rearrange("p (o d c) -> p o d c", o=2, d=2)
                S.op('dve', lambda e: e.memset(hw4[0:1, :, 1, :], 0.0), reads=[], writes=["hw"])
            ab, abk = absw.next()
            S.op('act', lambda e: e.activation(ab[:], hw[:], AF.Abs), reads=["hw"], writes=[abk])

            def nfn(e):
                inst = None
                for cc in range(4):
                    inst = e.matmul(k.bank(4 + cc), onesb[:], ab[:, cc * 512:(cc + 1) * 512], start=(blk == 0), stop=(blk == 15))
                return inst
            S.op('pe', nfn, reads=[abk, "onesbf"], writes=["ps4", "ps5", "ps6", "ps7"])
            hw4 = hw[:].rearrange("p (o d c) -> p o d c", o=2, d=2)
            hsv = hs[:, blk, :].rearrange("p (o c) -> p o c", o=2)
            hdv = hd[:, blk, :].rearrange("p (o c) -> p o c", o=2)
            S.op('dve', lambda e: e.tensor_tensor(hsv, hw4[:, :, 0, :], hw4[:, :, 1, :], ALU.add), reads=["hw"], writes=[f"hs{blk}"])
            S.op('pool', lambda e: e.tensor_tensor(hdv, hw4[:, :, 1, :], hw4[:, :, 0, :], ALU.subtract), reads=["hw"], writes=[f"hd{blk}"])
        rn = ph.tile([128, 1024], F32, "rn")
        ns4 = k.psum[:, 2048:4096].rearrange("p (o d c) -> p o d c", o=2, d=2)
        rnv = rn[:].rearrange("p (o c) -> p o c", o=2)
        nt = ph.tile([128, 1024], F32, "nt")
        ntv = nt[:].rearrange("p (o c) -> p o c", o=2)
        S.op('dve', lambda e: e.tensor_copy(ntv, ns4[:, :, 0, :]), reads=["ps4", "ps5", "ps6", "ps7"], writes=["nt"])
        S.op('dve', lambda e: e.tensor_tensor(ntv, ntv, ns4[:, :, 1, :], ALU.add), reads=["nt", "ps4", "ps5", "ps6", "ps7"], writes=["nt"])
        S.op('dve', lambda e: e.tensor_scalar(nt[:], nt[:], EPS, float(L), ALU.add, ALU.mult), reads=["nt"], writes=["nt"])
        S.op('dve', lambda e: e.reciprocal(rn[:], nt[:]), reads=["nt"], writes=["rn"])
        tr = ph.ring(2, [128, 2, 2048], BF16, "tab")
        ho = ph.ring(2, [128, 1024], F32, "ho")
        for m in range(16):
            tb, tbk = tr.next()
            for ci in range(2):
                S.dma('sp', tb[:, ci, :], k.tabF[ci, m], writes=[tbk + f".{ci}"])
            for ci, src in enumerate([hs, hd]):
                for half in range(2):
                    bk = ci * 2 + half
                    mmgroup(S, k.bank(bk), [(tb[:, ci, kk * 128:(kk + 1) * 128], src[:, kk, half * 512:(half + 1) * 512]) for kk in range(16)],
                            reads=[tbk + f".{ci}"] + [f"{'hs' if ci == 0 else 'hd'}{kk}" for kk in range(16)], wkeys=[f"ps{bk}"])
            for ci in range(2):
                h, hk = ho.next()
                S.op('dve', lambda e: e.tensor_tensor(h[:], k.psum[:, ci * 1024:(ci + 1) * 1024], rn[:], ALU.mult),
                     reads=[f"ps{2 * ci}", f"ps{2 * ci + 1}", "rn"], writes=[hk])
                S.dma('pool', k.Hs[ci, m], h[:], reads=[hk], writes=[f"Hs{m}"])


def stage_H(k):
    S, nc = k.S, k.nc
    with Phase(k, "H") as ph:
        HB = ph.tile([128, 2, 512], F32, "HB")
        for o in range(2):
            S.dma('sp', HB[:, o, :], bc_row(k.hy_bias[o:o + 1, :], 512), writes=[f"HB{o}"])
        hyG = ph.tile([128, 512], F32, "hyG")
        S.dma('sp', hyG[:], bc_row(k.hy_norm_g, 512), writes=["hyG"])
        curbf = ph.tile([128, 16, 512], BF16, "curbf")
        cur32 = ph.tile([128, 16, 512], F32, "cur32")
        W = ph.tile([128, 2, 16, 512], BF16, "W")
        tr = ph.ring(2, [128, 2, 2048], BF16, "tab")
        Hr = ph.ring(2, [128, 2, 512], F32, "Hri")
        ab = ph.ring(2, [128, 2, 512], F32, "ab")
        t1 = ph.tile([128, 512], F32, "t1")
        t2 = ph.tile([128, 512], F32, "t2")
        t3 = ph.tile([128, 512], F32, "t3")
        t4 = ph.tile([128, 512], F32, "t4")
        xgr = ph.ring(2, [128, 512], F32, "xg")
        tb_ = ph.tile([128, 512], F32, "tb")
        lc = ph.tile([128, 512], F32, "lc")
        zz = ph.ring(2, [128, 512], F32, "zz")
        junk = ph.tile([128, 512], BF16, "junk")
        sqr = ph.ring(2, [128, 4], F32, "sq")
        yb = ph.ring(2, [128, 512], BF16, "yb")
        for b in range(NB):
            T0 = b * L
            uv = k.U[T0:T0 + L, 0:512].rearrange("(k p) c -> p k c", p=128)
            for q in range(4):
                S.dma('pool', curbf[:, q * 4:(q + 1) * 4, :], uv[:, q * 4:(q + 1) * 4, :],
                      reads=[f"U{b}.{i}" for i in range(q * 4, q * 4 + 4)], writes=[f"curbf{i}" for i in range(q * 4, q * 4 + 4)])
                S.dma('sp', cur32[:, q * 4:(q + 1) * 4, :], uv[:, q * 4:(q + 1) * 4, :],
                      reads=[f"U{b}.{i}" for i in range(q * 4, q * 4 + 4)], writes=[f"cur32{i}" for i in range(q * 4, q * 4 + 4)])
            for o in range(2):
                for m in range(16):
                    tb, tbk = tr.next()
                    for ci in range(2):
                        S.dma('sp', tb[:, ci, :], k.tabF[ci, m], writes=[tbk + f".{ci}"])
                    h, hk = Hr.next()
                    for ci in range(2):
                        S.dma('sp', h[:, ci, :], k.Hs[ci, m][:, o * 512:(o + 1) * 512], reads=[f"Hs{m}"], writes=[hk + f".{ci}"])
                    for ci in range(2):
                        bk = (m % 2) * 2 + ci
                        mmgroup(S, k.bank(bk), [(tb[:, ci, kk * 128:(kk + 1) * 128], curbf[:, kk, :]) for kk in range(16)],
                                reads=[tbk + f".{ci}"] + [f"curbf{kk}" for kk in range(16)], wkeys=[f"ps{bk}"])
                    a_, ak = ab.next()
                    for ci in range(2):
                        bk = (m % 2) * 2 + ci
                        S.op('act', lambda e: e.copy(a_[:, ci, :], k.bank(bk)), reads=[f"ps{bk}"], writes=[ak + f".{ci}"])
                    S.op('dve', lambda e: e.tensor_tensor(t1[:], a_[:, 0, :], h[:, 0, :], ALU.mult), reads=[ak + ".0", hk + ".0"], writes=["H.t1"])
                    S.op('pool', lambda e: e.tensor_tensor(t2[:], a_[:, 1, :], h[:, 1, :], ALU.mult), reads=[ak + ".1", hk + ".1"], writes=["H.t2"])
                    S.op('dve', lambda e: e.tensor_tensor(W[:, 0, m, :], t1[:], t2[:], ALU.add), reads=["H.t1", "H.t2"], writes=[f"Wr{m}"])
                    S.op('pool', lambda e: e.tensor_tensor(t3[:], a_[:, 1, :], h[:, 0, :], ALU.mult), reads=[ak + ".1", hk + ".0"], writes=["H.t3"])
                    S.op('dve', lambda e: e.tensor_tensor(t4[:], a_[:, 0, :], h[:, 1, :], ALU.mult), reads=[ak + ".0", hk + ".1"], writes=["H.t4"])
                    S.op('pool', lambda e: e.tensor_tensor(W[:, 1, m, :], t3[:], t4[:], ALU.subtract), reads=["H.t3", "H.t4"], writes=[f"Wi{m}"])
                for m in range(16):
                    tb, tbk = tr.next()
                    for ci in range(2):
                        S.dma('sp', tb[:, ci, :], k.tabI[ci, m], writes=[tbk + f".{ci}"])
                    xg, xgk = xgr.next()
                    r0 = T0 + m * 128
                    S.dma('sp', xg[:], k.U[r0:r0 + 128, (o + 1) * 512:(o + 2) * 512], reads=[f"U{b}.{m}"], writes=[xgk])
                    bk = 4 + (m % 2)
                    pairs = [(tb[:, ci, kk * 128:(kk + 1) * 128], W[:, ci, kk, :]) for ci in range(2) for kk in range(16)]
                    mmgroup(S, k.bank(bk), pairs,
                            reads=[tbk + ".0", tbk + ".1"] + [f"Wr{kk}" for kk in range(16)] + [f"Wi{kk}" for kk in range(16)],
                            wkeys=[f"ps{bk}"])
                    S.op('pool', lambda e: e.tensor_tensor(tb_[:], cur32[:, m, :], HB[:, o, :], ALU.mult),
                         reads=[f"cur32{m}", f"HB{o}"], writes=["H.tb"])
                    S.op('dve', lambda e: e.tensor_tensor(lc[:], k.bank(bk), tb_[:], ALU.add), reads=[f"ps{bk}", "H.tb"], writes=["H.lc"])
                    if o == 0:
                        S.op('pool', lambda e: e.tensor_tensor(cur32[:, m, :], lc[:], xg[:], ALU.mult),
                             reads=["H.lc", xgk], writes=[f"cur32{m}"])
                        S.op('act', lambda e: e.copy(curbf[:, m, :], cur32[:, m, :]), reads=[f"cur32{m}"], writes=[f"curbf{m}"])
                    else:
                        z, zk = zz.next()
                        S.op('pool', lambda e: e.tensor_tensor(z[:], lc[:], xg[:], ALU.mult), reads=["H.lc", xgk], writes=[zk])
                        sq, sqk = sqr.next()
                        rstd = rms_rstd(k, z[:], [zk], junk[:], "H.junk", sq, sqk, 512)
                        y, yk = yb.next()
                        S.op('dve', lambda e: e.scalar_tensor_tensor(y[:], z[:], rstd, hyG[:], ALU.mult, ALU.mult),
                             reads=[zk, sqk, "hyG"], writes=[yk])
                        S.dma('pool', k.Ymix[r0:r0 + 128, 0:512], y[:], reads=[yk], writes=[f"Ymix{b}.{m}a"])


def run_window(gens, K):
    active = []
    it = iter(gens)
    more = True
    while True:
        if len(active) < K and more:
            try:
                active.append(next(it))
            except StopIteration:
                more = False
        if not active:
            break
        for g in list(active):
            try:
                next(g)
            except StopIteration:
                active.remove(g)


def stage_G(k):
    S, nc = k.S, k.nc
    KW = int(os.environ.get("G_WIN", "4"))
    with Phase(k, "G") as ph:
        lbt = ph.tile([128, 2, 1024], F32, "lbraw")
        for l_ in range(2):
            S.dma('sp', lbt[:, l_, :], bc_row(k.hg_lb[l_:l_ + 1, :], 1024), writes=[f"lbraw{l_}"])
        lb = ph.tile([128, 1024], F32, "lb")
        oml = ph.tile([128, 1024], F32, "oml")
        S.op('dve', lambda e: e.tensor_tensor(lb[:], lbt[:, 0, :], lbt[:, 1, :], ALU.subtract), reads=["lbraw0", "lbraw1"], writes=["lb"])
        S.op('act', lambda e: e.activation(lb[:], lb[:], AF.Sigmoid), reads=["lb"], writes=["lb"])
        S.op('dve', lambda e: e.tensor_scalar(oml[:], lb[:], -1.0, 1.0, ALU.mult, ALU.add), reads=["lb"], writes=["oml"])
        hgG = ph.tile([128, 4, 128], F32, "hgG")
        for h in range(4):
            S.dma('sp', hgG[:, h, :], bc_row(k.hg_norm_g, 128), writes=[f"hgG{h}"])
        hgGk = [f"hgG{h}" for h in range(4)]
        selt = ph.tile([128, 2, 4], F32, "selt")
        for d in range(2):
            S.dma('sp', selt[:, d, :], k.sel[d], writes=[f"selt{d}"])
        cum = [k.c32["cumf"], k.c32["cumb"]]
        cumk = ["cumf32", "cumb32"]
        mask = [k.c32["maskf"], k.c32["maskb"]]
        maskk = ["maskf32", "maskb32"]
        identb = k.cbf["ident"]
        rowm = k.smallt[:, 258:260]
        Sst = [ph.tile([128, 512], F32, f"S{b}") for b in range(NB)]
        NQ = KW
        qzs = []
        for i in range(NQ):
            pair = []
            for j in range(2):
                t = ph.tile([128, 4, 128], BF16, f"qz{i}{j}")
                S.op('pool', lambda e: e.memset(t[:], 0.0), writes=[f"qz{i}.{j}"])
                pair.append(t)
            qzs.append(pair)
        qzi = [0]
        psi = [0]

        def psnext():
            i = psi[0] % 8
            psi[0] += 1
            return k.bank(i), f"ps{i}"

        def R(shape, dt, name, n=None):
            return ph.ring(n or KW, shape, dt, name)
        zr, kkr, vr, qr, Er, Eir = (R([128, 512], F32, n_) for n_ in ("z", "kk", "v", "q", "E", "Ei"))
        qtr, ktr, vbr, kz0r, kz1r = (R([128, 512], BF16, n_) for n_ in ("qt", "kt", "vb", "kz0", "kz1"))
        ATr, kTr = R([128, 4, 128], BF16, "AT"), R([128, 2, 4, 128], BF16, "kT")
        ecr, dltr, selsbr = R([128, 4, 4], F32, "ec"), R([128, 4, 2], F32, "dlt"), R([128, 4, 4], F32, "selsb")
        Spr = R([128, 512], BF16, "Sp", 2 * KW)
        tmpr, ofr, gr, orr, sqr2 = (R([128, 512], F32, n_) for n_ in ("tmp", "of", "g", "o", "sq"))
        ssr, ybr = R([128, 8], F32, "ss"), R([128, 512], BF16, "yb")
        qscale = 128.0 ** -0.5
        turn = [0, 0]
        onesf = ph.tile([128, 512], F32, "onesf")
        S.op('pool', lambda e: e.memset(onesf[:], 1.0), writes=["onesf"])

        def unit(d, lat, blk, b, idx):
            if lat:
                r0 = b * L + blk * 128
                src, srcp, zc, vc = k.Phg, f"Phg{b}.{blk}.", (2 + d) * 512, 512
            else:
                r0 = b * CTX + blk * 128
                src, srcp, zc, vc = k.Pctx, f"Pctx{b}.{blk}.", (1 + d) * 512, 0
            z, zk = zr.next()
            v, vk = vr.next()
            S.dma('sp', z[:], src[r0:r0 + 128, zc:zc + 512], reads=[srcp + str(zc // 512)], writes=[zk])
            S.dma('sp', v[:], src[r0:r0 + 128, vc:vc + 512], reads=[srcp + str(vc // 512)], writes=[vk])
            if lat:
                q, qk = qr.next()
                S.dma('sp', q[:], src[r0:r0 + 128, 0:512], reads=[srcp + "0"], writes=[qk])
                if d == 1:
                    of_, ofk = ofr.next()
                    S.dma('sp', of_[:], k.Of[r0:r0 + 128, :], reads=[f"Of{b}.{blk}"], writes=[ofk])
                    g, gk = gr.next()
                    S.dma('sp', g[:], k.Phg[r0:r0 + 128, 2048:2560], reads=[srcp + "4"], writes=[gk])
            yield
            S.op('act', lambda e: e.activation(z[:], z[:], AF.Sigmoid), reads=[zk], writes=[zk])
            yield
            S.op('dve', lambda e: e.tensor_tensor(z[:], z[:], oml[:, d * 512:(d + 1) * 512], ALU.mult), reads=[zk, "oml"], writes=[zk])
            yield
            S.op('pool', lambda e: e.tensor_tensor(z[:], z[:], lb[:, d * 512:(d + 1) * 512], ALU.add), reads=[zk, "lb"], writes=[zk])
            yield
            kk_, kkk = kkr.next()
            S.op('pool', lambda e: e.tensor_tensor(kk_[:], onesf[:], z[:], ALU.subtract), reads=[zk, "onesf"], writes=[kkk])
            S.op('act', lambda e: e.activation(z[:], z[:], AF.Ln), reads=[zk], writes=[zk])
            lf, lfk = z, zk
            vb, vbk = vbr.next()
            S.op('pool', lambda e: e.tensor_copy(vb[:], v[:]), reads=[vk], writes=[vbk])
            if lat:
                S.op('act', lambda e: e.activation(q[:], q[:], AF.Silu), reads=[qk], writes=[qk])
                if d == 1:
                    S.op('act', lambda e: e.activation(g[:], g[:], AF.Silu), reads=[gk], writes=[gk])
            yield
            cb, cbk = psnext()
            mmgroup(S, cb, [(cum[d][:], lf[:])], reads=[cumk[d], lfk], wkeys=[cbk])
            sb_, sbk = psnext()
            selps = sb_[:, 0:16].rearrange("p (h c) -> p h c", h=4)

            def self_(e):
                inst = None
                for h in range(4):
                    inst = e.matmul(selps[:, h, :], lf[:, h * 128:(h + 1) * 128], selt[:, d, :], start=True, stop=True)
                return inst
            S.op('pe', self_, reads=[lfk, f"selt{d}"], writes=[sbk])
            Ei, Eik = Eir.next()
            S.op('act', lambda e: e.activation(Ei[:], cb, AF.Exp, scale=-1.0), reads=[cbk], writes=[Eik])
            if lat:
                E, Ek = Er.next()
                S.op('act', lambda e: e.activation(E[:], cb, AF.Exp), reads=[cbk], writes=[Ek])
            ssb, ssbk = selsbr.next()
            S.op('act', lambda e: e.copy(ssb[:], selps), reads=[sbk], writes=[ssbk])
            yield
            ec, eck = ecr.next()
            dlt, dltk = dltr.next()
            S.op('act', lambda e: e.activation(ec[:], ssb[:], AF.Exp), reads=[ssbk], writes=[eck])
            spv = ssb[:].rearrange("p h (j t) -> p h j t", j=2)
            S.op('dve', lambda e: e.tensor_tensor(dlt[:], spv[:, :, :, 1], spv[:, :, :, 0], ALU.subtract), reads=[ssbk], writes=[dltk])
            kt, ktk = ktr.next()
            S.op('dve', lambda e: e.tensor_tensor(kt[:], kk_[:], Ei[:], ALU.mult), reads=[kkk, Eik], writes=[ktk])
            if lat:
                qt, qtk = qtr.next()
                S.op('dve', lambda e: e.scalar_tensor_tensor(qt[:], q[:], qscale, E[:], ALU.mult, ALU.mult), reads=[qk, Ek], writes=[qtk])
            yield
            S.op('act', lambda e: e.activation(dlt[:], dlt[:], AF.Exp), reads=[dltk], writes=[dltk])
            kz = []
            for j, rr_ in enumerate([kz0r, kz1r]):
                kzt, kzk = rr_.next()
                if j == 0:
                    S.op('act', lambda e: e.activation(kzt[:], kt[:], AF.Copy, scale=rowm[:, j:j + 1]), reads=[ktk, "small"], writes=[kzk])
                else:
                    S.op('dve', lambda e: e.tensor_single_scalar(kzt[:], kt[:], rowm[:, j:j + 1], ALU.mult), reads=[ktk, "small"], writes=[kzk])
                kz.append((kzt, kzk))
            yield
            if lat:
                qb_, qbk = psnext()
                psq = qb_.bitcast(BF16)

                def trq(e):
                    inst = None
                    for h in range(4):
                        inst = e.transpose(psq[:, h * 128:(h + 1) * 128], qt[:, h * 128:(h + 1) * 128], identb[:])
                    for h in range(4):
                        inst = e.transpose(psq[:, 512 + h * 128:512 + (h + 1) * 128], kt[:, h * 128:(h + 1) * 128], identb[:])
                    return inst
                S.op('pe', trq, reads=[qtk, ktk, "identbf"], writes=[qbk])
                kT, kTk = kTr.next()
                S.op('act', lambda e: e.copy(kT[:].rearrange("p a h t -> p (a h t)"), psq), reads=[qbk], writes=[kTk])
                yield
                qi = qzi[0] % NQ
                qzi[0] += 1
                qz, qzk = qzs[qi], [f"qz{qi}.0", f"qz{qi}.1"]
                S.op('pool', lambda e: e.tensor_copy(qz[0][:, :, 0:64], kT[:, 0, :, 0:64]), reads=[kTk], writes=[qzk[0]])
                S.op('pool', lambda e: e.tensor_copy(qz[1][:, :, 64:128], kT[:, 0, :, 64:128]), reads=[kTk], writes=[qzk[1]])
                ab_, abk = psnext()
                atps = ab_.rearrange("p (h t) -> p h t", h=4)

                def atf(e):
                    inst = None
                    for h in range(4):
                        inst = e.matmul(atps[:, h, :], kT[:, 1, h, :], kT[:, 0, h, :], start=True, stop=True)
                    return inst
                S.op('pe', atf, reads=[kTk], writes=[abk])
                AT, ATk = ATr.next()
                S.op('dve', lambda e: e.tensor_tensor(AT[:], atps, mask[d][:].unsqueeze(1).to_broadcast([128, 4, 128]), ALU.mult),
                     reads=[abk, maskk[d]], writes=[ATk])
                yield
            while turn[b] != idx:
                yield
            Sk, St = f"S{b}", Sst[b]
            ecv = ec[:].rearrange("p h (j t) -> p h j t", j=2)
            S4 = St[:].rearrange("p (h v) -> p h v", h=4)
            Sps = {}
            for jj, j in enumerate([0, 1] if d == 0 else [1, 0]):
                ecj = ecv[:, :, j, 0:1].to_broadcast([128, 4, 128])
                e1j = ecv[:, :, j, 1:2].to_broadcast([128, 4, 128])
                e2j = dlt[:, :, j:j + 1].to_broadcast([128, 4, 128])
                if lat:
                    Sp, Spk = Spr.next()
                    S.op('dve', lambda e: e.tensor_tensor(Sp[:].rearrange("p (h v) -> p h v", h=4), S4, ecj, ALU.mult),
                         reads=[Sk + f".{h}" for h in range(4)] + [eck], writes=[Spk])
                    Sps[j] = (Sp, Spk)
                kzt, kzk = kz[j]
                stps, stk = psnext()

                def stf(e):
                    inst = None
                    for h in range(4):
                        inst = e.matmul(stps[:, h * 128:(h + 1) * 128], kzt[:, h * 128:(h + 1) * 128], vb[:, h * 128:(h + 1) * 128],
                                        start=True, stop=True)
                    return inst
                S.op('pe', stf, reads=[kzk, vbk], writes=[stk])
                tmp, tmpk = tmpr.next()
                S.op('dve', lambda e: e.tensor_tensor(tmp[:].rearrange("p (h v) -> p h v", h=4), stps.rearrange("p (h v) -> p h v", h=4), e2j, ALU.mult),
                     reads=[stk, dltk], writes=[tmpk])
                for h in range(4):
                    hs_ = slice(h * 128, (h + 1) * 128)
                    S.op('dve', lambda e: e.scalar_tensor_tensor(St[:, hs_], St[:, hs_], ecv[:, h, j, 1:2], tmp[:, hs_], ALU.mult, ALU.add),
                         reads=[Sk + f".{h}", eck, tmpk], writes=[Sk + f".{h}"])
                if jj == 1:
                    turn[b] += 1
                yield
            if not lat:
                return
            ops_, opk = psnext()

            def ofn(e):
                inst = None
                for h in range(4):
                    hs_ = slice(h * 128, (h + 1) * 128)
                    e.matmul(ops_[:, hs_], AT[:, h, :], vb[:, hs_], start=True, stop=False)
                    e.matmul(ops_[:, hs_], qz[0][:, h, :], Sps[0][0][:, hs_], start=False, stop=False)
                    inst = e.matmul(ops_[:, hs_], qz[1][:, h, :], Sps[1][0][:, hs_], start=False, stop=True)
                return inst
            S.op('pe', ofn, reads=[ATk, vbk, Sps[0][1], Sps[1][1]] + qzk, writes=[opk])
            if d == 0:
                of_, ofk = ofr.next()
                S.op('act', lambda e: e.copy(of_[:], ops_), reads=[opk], writes=[ofk])
                S.dma('pool', k.Of[r0:r0 + 128, :], of_[:], reads=[ofk], writes=[f"Of{b}.{blk}"])
                return
            o, ok_ = orr.next()
            S.op('dve', lambda e: e.tensor_tensor(o[:], ops_, of_[:], ALU.add), reads=[opk, ofk], writes=[ok_])
            yield
            sq, sqk = sqr2.next()
            S.op('pool', lambda e: e.tensor_tensor(sq[:], o[:], o[:], ALU.mult), reads=[ok_], writes=[sqk])
            yield
            ss, ssk = ssr.next()
            S.op('dve', lambda e: e.tensor_reduce(ss[:, 0:4], sq[:].rearrange("p (h v) -> p h v", h=4), AX.X, ALU.add), reads=[sqk], writes=[ssk + "a"])
            yield
            S.op('act', lambda e: e.activation(ss[:, 0:4], ss[:, 0:4], AF.Sqrt, scale=1.0 / 128, bias=EPS), reads=[ssk + "a"], writes=[ssk + "a"])
            yield
            S.op('dve', lambda e: e.reciprocal(ss[:, 4:8], ss[:, 0:4]), reads=[ssk + "a"], writes=[ssk])
            S.op('dve', lambda e: e.tensor_tensor(o[:].rearrange("p (h v) -> p h v", h=4), o[:].rearrange("p (h v) -> p h v", h=4),
                                                  ss[:, 4:8].unsqueeze(2).to_broadcast([128, 4, 128]), ALU.mult), reads=[ok_, ssk], writes=[ok_])
            yield
            S.op('pool', lambda e: e.tensor_tensor(o[:], o[:], hgG[:].rearrange("p h v -> p (h v)"), ALU.mult), reads=[ok_] + hgGk, writes=[ok_])
            yield
            y, yk = ybr.next()
            S.op('dve', lambda e: e.tensor_tensor(y[:], o[:], g[:], ALU.mult), reads=[ok_, gk], writes=[yk])
            S.dma('pool', k.Ymix[r0:r0 + 128, 512:1024], y[:], reads=[yk], writes=[f"Ymix{b}.{blk}b"])

        for d in range(2):
            for b in range(NB):
                S.op('pool', lambda e: e.memset(Sst[b][:], 0.0), writes=[f"S{b}.{h}" for h in range(4)])
            turn[0] = turn[1] = 0
            order = [(False, i) for i in ([0, 1] if d == 0 else [1, 0])] + [(True, i) for i in (range(16) if d == 0 else range(15, -1, -1))]
            gens = (unit(d, lat, blk, b, idx) for idx, (lat, blk) in enumerate(order) for b in range(NB))
            run_window(gens, KW)


def stage_O(k):
    S, nc = k.S, k.nc
    KW = int(os.environ.get("O_WIN", "3"))
    with Phase(k, "O") as ph:
        wout = ph.tile([128, 8, D], BF16, "wout")
        wv = k.w_out.rearrange("(k p) n -> p k n", p=128)
        for h in range(2):
            S.dma('pool', wout[:, :, h * 512:(h + 1) * 512], wv[:, :, h * 512:(h + 1) * 512], writes=[f"wout{h}"])
        rw = ph.tile([128, 8, NE], F32, "rw")
        S.dma('sp', rw[:], k.router_w.rearrange("(k p) n -> p k n", p=128), writes=["rw"])
        G1 = [load_mod(k, ph, b, 2, f"G1_{b}") for b in range(NB)]
        A2 = [make_scale(k, ph, b, 4, k.norm2_g, f"A2_{b}") for b in range(NB)]
        B2 = [load_mod(k, ph, b, 3, f"B2_{b}") for b in range(NB)]
        ymr = ph.ring(KW, [128, D], BF16, "ym")
        ymTr = ph.ring(KW, [128, 8, 128], BF16, "ymT")
        xr = ph.ring(KW, [128, D], F32, "x")
        ttr = ph.ring(KW, [128, D], F32, "tt")
        xmr = ph.ring(KW, [128, D], F32, "xm")
        junk = ph.tile([128, D], BF16, "junk")
        sqr = ph.ring(KW, [128, 4], F32, "sq")
        h2r = ph.ring(KW, [128, D], F32, "h2")
        h2br = ph.ring(KW, [128, D], BF16, "h2b")
        h2Tr = ph.ring(KW, [128, 8, 128], F32, "h2T")
        smr = ph.ring(KW, [128, 4], F32, "sm")
        er = ph.ring(KW, [128, NE], F32, "e")
        identb = k.cbf["ident"]
        ident32 = k.c32["ident"]
        psT = k.bank(7).bitcast(BF16)
        psR = k.psum[:, 2 * 512:4 * 512]

        def unit(b, blk):
            r0 = b * L + blk * 128
            ym, ymk = ymr.next()
            S.dma('sp', ym[:], k.Ymix[r0:r0 + 128, :], reads=[f"Ymix{b}.{blk}a", f"Ymix{b}.{blk}b"], writes=[ymk])
            xt, xk = xr.next()
            S.dma('sp', xt[:], k.x[r0:r0 + 128, :], writes=[xk])
            yield

            def trs(e):
                inst = None
                for kk in range(8):
                    inst = e.transpose(psT[:, kk * 128:(kk + 1) * 128], ym[:, kk * 128:(kk + 1) * 128], identb[:])
                return inst
            S.op('pe', trs, reads=[ymk, "identbf"], writes=["ps7"])
            ymT, ymTk = ymTr.next()
            S.op('act', lambda e: e.copy(ymT[:].rearrange("p a b -> p (a b)"), psT), reads=["ps7"], writes=[ymTk])
            yield
            for h in range(2):
                mmgroup(S, k.bank(h), [(ymT[:, kk, :], wout[:, kk, h * 512:(h + 1) * 512]) for kk in range(8)],
                        reads=[ymTk, f"wout{h}"], wkeys=[f"ps{h}"])
            tt, ttk = ttr.next()
            S.op('dve', lambda e: e.tensor_tensor(tt[:], k.psum[:, 0:1024], G1[b][:], ALU.mult), reads=["ps0", "ps1", f"G1_{b}"], writes=[ttk])
            yield
            xm, xmk = xmr.next()
            S.op('pool', lambda e: e.tensor_tensor(xm[:], tt[:], xt[:], ALU.add), reads=[ttk, xk], writes=[xmk])
            S.dma('pool', k.Xmid[r0:r0 + 128, :], xm[:], reads=[xmk], writes=[f"Xmid{b}.{blk}"])
            yield
            sq, sqk = sqr.next()
            S.op('act', lambda e: e.activation(junk[:], xm[:], AF.Square, accum_out=sq[:, 0:1]), reads=[xmk], writes=["O.junk", sqk + "a"])
            yield
            S.op('act', lambda e: e.activation(sq[:, 1:2], sq[:, 0:1], AF.Sqrt, scale=1.0 / D, bias=EPS), reads=[sqk + "a"], writes=[sqk + "b"])
            yield
            S.op('dve', lambda e: e.reciprocal(sq[:, 2:3], sq[:, 1:2]), reads=[sqk + "b"], writes=[sqk])
            h2, h2k = h2r.next()
            S.op('dve', lambda e: e.scalar_tensor_tensor(h2[:], xm[:], sq[:, 2:3], A2[b][:], ALU.mult, ALU.mult), reads=[xmk, sqk, f"A2_{b}"], writes=[h2k])
            yield
            S.op('pool', lambda e: e.tensor_tensor(h2[:], h2[:], B2[b][:], ALU.add), reads=[h2k, f"B2_{b}"], writes=[h2k])
            yield
            h2b, h2bk = h2br.next()
            S.op('act', lambda e: e.copy(h2b[:], h2[:]), reads=[h2k], writes=[h2bk])
            S.dma('pool', k.H2[r0:r0 + 128, :], h2b[:], reads=[h2bk], writes=[f"H2{b}.{blk}"])

            def trr(e):
                inst = None
                for kk in range(8):
                    inst = e.transpose(psR[:, kk * 128:(kk + 1) * 128], h2[:, kk * 128:(kk + 1) * 128], ident32[:])
                return inst
            S.op('pe', trr, reads=[h2k, "ident32"], writes=["ps2", "ps3"])
            h2T, h2Tk = h2Tr.next()
            h2Tf = h2T[:].rearrange("p a b -> p (a b)")
            S.op('act', lambda e: e.copy(h2Tf[:, 0:512], psR[:, 0:512]), reads=["ps2"], writes=[h2Tk + ".0"])
            S.op('dve', lambda e: e.tensor_copy(h2Tf[:, 512:1024], psR[:, 512:1024]), reads=["ps3"], writes=[h2Tk + ".1"])
            yield
            lg = k.bank(4)[:, 0:NE]
            mmgroup(S, lg, [(h2T[:, kk, :], rw[:, kk, :]) for kk in range(8)], reads=[h2Tk + ".0", h2Tk + ".1", "rw"], wkeys=["ps4"])
            sm, smk = smr.next()
            S.op('dve', lambda e: e.tensor_reduce(sm[:, 0:1], lg, AX.X, ALU.max), reads=["ps4"], writes=[smk + "a"])
            S.op('dve', lambda e: e.tensor_single_scalar(sm[:, 1:2], sm[:, 0:1], -1.0, ALU.mult), reads=[smk + "a"], writes=[smk + "b"])
            ee, eek = er.next()
            S.op('act', lambda e: e.activation(ee[:], lg, AF.Exp, bias=sm[:, 1:2], scale=1.0, accum_out=sm[:, 2:3]),
                 reads=["ps4", smk + "b"], writes=[eek, smk + "c"])
            yield
            S.op('dve', lambda e: e.reciprocal(sm[:, 3:4], sm[:, 2:3]), reads=[smk + "c"], writes=[smk + "d"])
            S.op('dve', lambda e: e.tensor_single_scalar(k.aff[:, b, blk, :], ee[:], sm[:, 3:4], ALU.mult), reads=[eek, smk + "d"], writes=[f"aff{b}.{blk}"])

        run_window((unit(b, blk) for b in range(NB) for blk in range(16)), KW)
        if k.debug:
            S.dma('pool', k.dbg_aff, k.aff[:].rearrange("p b k e -> p (b k e)"), reads=[f"aff{b}.{blk}" for b in range(NB) for blk in range(16)], writes=["dbg_aff"])


def stage_R(k):
    S, nc = k.S, k.nc
    affk = [f"aff{b}.{blk}" for b in range(NB) for blk in range(16)]
    with Phase(k, "R") as ph:
        lo = ph.tile([128, NB, NE], F32, "lo")
        hi = ph.tile([128, NB, NE], F32, "hi")
        mid = ph.tile([128, NB, NE], F32, "mid")
        cmp_ = ph.tile([128, NB, 16, NE], F32, "cmp")
        cp = ph.tile([128, NB, NE], F32, "cp")
        ge = ph.tile([128, NB, NE], F32, "ge")
        d1 = ph.tile([128, NB, NE], F32, "d1")
        d2 = ph.tile([128, NB, NE], F32, "d2")
        ones32 = k.c32["ones"]
        S.op('dve', lambda e: e.memset(lo[:], 0.0), writes=["lo"])
        S.op('dve', lambda e: e.memset(hi[:], 1.0), writes=["hi"])
        cps = k.bank(0)[:, 0:NB * NE]
        f2 = lambda t: t[:].rearrange("p b e -> p (b e)")
        for it in range(26):
            S.op('dve', lambda e: e.tensor_tensor(mid[:], lo[:], hi[:], ALU.add), reads=["lo", "hi"], writes=["mid"])
            S.op('dve', lambda e: e.tensor_single_scalar(mid[:], mid[:], 0.5, ALU.mult), reads=["mid"], writes=["mid"])
            S.op('dve', lambda e: e.tensor_tensor(cmp_[:], k.aff[:], mid[:].unsqueeze(2).to_broadcast([128, NB, 16, NE]), ALU.is_gt),
                 reads=affk + ["mid"], writes=["cmp"])
            S.op('dve', lambda e: e.tensor_reduce(cp[:], cmp_[:].rearrange("p b k e -> p b e k"), AX.X, ALU.add), reads=["cmp"], writes=["cp"])
            mmgroup(S, cps, [(ones32[:], f2(cp))], reads=["cp", "ones32"], wkeys=["ps0"])
            S.op('dve', lambda e: e.tensor_single_scalar(f2(ge), cps, float(CAP) - 0.5, ALU.is_gt), reads=["ps0"], writes=["ge"])
            S.op('dve', lambda e: e.tensor_tensor(d1[:], mid[:], lo[:], ALU.subtract), reads=["mid", "lo"], writes=["d1"])
            S.op('dve', lambda e: e.tensor_tensor(d1[:], d1[:], ge[:], ALU.mult), reads=["d1", "ge"], writes=["d1"])
            S.op('dve', lambda e: e.tensor_tensor(d2[:], hi[:], mid[:], ALU.subtract), reads=["hi", "mid"], writes=["d2"])
            S.op('dve', lambda e: e.tensor_tensor(d2[:], d2[:], ge[:], ALU.mult), reads=["d2", "ge"], writes=["d2"])
            S.op('dve', lambda e: e.tensor_tensor(lo[:], lo[:], d1[:], ALU.add), reads=["lo", "d1"], writes=["lo"])
            S.op('dve', lambda e: e.tensor_tensor(hi[:], mid[:], d2[:], ALU.add), reads=["mid", "d2"], writes=["hi"])
        maskb = ph.tile([128, NB, 16, NE], BF16, "maskb")
        S.op('dve', lambda e: e.tensor_tensor(cmp_[:], k.aff[:], lo[:].unsqueeze(2).to_broadcast([128, NB, 16, NE]), ALU.is_gt),
             reads=affk + ["lo"], writes=["cmp"])
        S.op('act', lambda e: e.copy(maskb[:], cmp_[:]), reads=["cmp"], writes=["maskb"])
        pre = k.bank(1).rearrange("p (k b e) -> p k b e", k=16, b=NB)
        onesb, trib = k.cbf["ones"], k.cbf["tri"]

        def pf(e):
            inst = None
            for blk in range(16):
                for bb in range(NB):
                    for pb in range(blk + 1):
                        l = trib if pb == blk else onesb
                        inst = e.matmul(pre[:, blk, bb, :], l[:], maskb[:, bb, pb, :], start=(pb == 0), stop=(pb == blk))
            return inst
        S.op('pe', pf, reads=["maskb", "onesbf", "tribf"], writes=["ps1"])
        v1 = ph.tile([128, NB, 16, NE], F32, "v1")
        for bb in range(NB):
            S.op('dve', lambda e: e.scalar_tensor_tensor(v1[:, bb], pre[:, :, bb, :], 1.0, cmp_[:, bb], ALU.add, ALU.mult),
                 reads=["ps1", "cmp"], writes=["v1"])
        S.op('dve', lambda e: e.tensor_single_scalar(k.val[:], v1[:], -1.0, ALU.add), reads=["v1"], writes=["val"])
        ahi = ph.tile([128, NB, 16, NE], BF16, "ahi")
        a32 = ph.tile([128, NB, 16, NE], F32, "a32")
        S.op('act', lambda e: e.copy(ahi[:], k.aff[:]), reads=affk, writes=["ahi"])
        S.op('dve', lambda e: e.tensor_copy(k.affhl[:, :, :, :, 0], ahi[:]), reads=["ahi"], writes=["affhl0"])
        S.op('dve', lambda e: e.tensor_tensor(a32[:], k.aff[:], ahi[:], ALU.subtract), reads=affk + ["ahi"], writes=["a32"])
        S.op('dve', lambda e: e.tensor_copy(k.affhl[:, :, :, :, 1], a32[:]), reads=["a32"], writes=["affhl1"])
        if k.debug:
            S.dma('pool', k.dbg_val, k.val[:].rearrange("p b k e -> p (b k e)"), reads=["val"], writes=["dbg_val"])


def stage_XG(k):
    S, nc = k.S, k.nc
    with Phase(k, "XG") as ph:
        h2 = ph.tile([128, 16, D], BF16, "h2")
        Pr = ph.ring(2, [128, 16, 2, CAP], BF16, "P")
        xgr = ph.ring(2, [128, 2, 8, CAP], BF16, "xg")
        gtr = ph.ring(2, [128, 4, 2], F32, "gt")
        iota = k.smallt[:, 0:256]
        for b in range(NB):
            hv = k.H2[b * L:(b + 1) * L, :].rearrange("(k p) c -> p k c", p=128)
            for q in range(4):
                S.dma('sp', h2[:, q * 4:(q + 1) * 4, :], hv[:, q * 4:(q + 1) * 4, :],
                      reads=[f"H2{b}.{i}" for i in range(q * 4, q * 4 + 4)], writes=[f"h2_{i}" for i in range(q * 4, q * 4 + 4)])
            h2k = [f"h2_{i}" for i in range(16)]
            for ep in range(NE // 2):
                P_, Pk = Pr.next()
                for e2 in range(2):
                    S.op('dve', lambda e: e.tensor_tensor(P_[:, :, e2, :], iota.unsqueeze(1).to_broadcast([128, 16, CAP]),
                                                          k.val[:, b, :, 2 * ep + e2].unsqueeze(2).to_broadcast([128, 16, CAP]), ALU.is_equal),
                         reads=["val", "small"], writes=[Pk + f".{e2}"])
                xg, xgk = xgr.next()
                for db in range(8):
                    bk = db % 4
                    mmgroup(S, k.bank(bk), [(h2[:, blk, db * 128:(db + 1) * 128], P_[:, blk, :, :].rearrange("p a c -> p (a c)")) for blk in range(16)],
                            reads=h2k + [Pk + ".0", Pk + ".1"], wkeys=[f"ps{bk}"])
                    eng = 'act' if db % 2 == 0 else 'dve'
                    if eng == 'act':
                        S.op('act', lambda e: e.copy(xg[:, :, db, :], k.bank(bk).rearrange("p (a c) -> p a c", a=2)), reads=[f"ps{bk}"], writes=[xgk + f".{db}"])
                    else:
                        S.op('dve', lambda e: e.tensor_copy(xg[:, :, db, :], k.bank(bk).rearrange("p (a c) -> p a c", a=2)), reads=[f"ps{bk}"], writes=[xgk + f".{db}"])
                for e2 in range(2):
                    S.dma('pool', k.XG[2 * ep + e2][:, :, b * CAP:(b + 1) * CAP], xg[:, e2, :, :],
                          reads=[xgk + f".{db}" for db in range(8)], writes=[f"XG{2 * ep + e2}"])
                gps = k.bank(4 + ep % 2)[:, 0:8].rearrange("p (a c) -> p a c", a=4)

                def gf(e):
                    inst = None
                    for e2 in range(2):
                        for ch in range(2):
                            for blk in range(16):
                                inst = e.matmul(gps[:, e2 * 2 + ch, :], P_[:, blk, e2, ch * 128:(ch + 1) * 128],
                                                k.affhl[:, b, blk, 2 * ep + e2, :], start=(blk == 0), stop=(blk == 15))
                    return inst
                S.op('pe', gf, reads=[Pk + ".0", Pk + ".1", "affhl0", "affhl1"], writes=[f"ps{4 + ep % 2}"])
                gt, gtk = gtr.next()
                S.op('act', lambda e: e.copy(gt[:], gps), reads=[f"ps{4 + ep % 2}"], writes=[gtk])
                for e2 in range(2):
                    S.op('dve', lambda e: e.tensor_tensor(k.gates[:, 2 * ep + e2, b, :], gt[:, e2 * 2:e2 * 2 + 2, 0], gt[:, e2 * 2:e2 * 2 + 2, 1], ALU.add),
                         reads=[gtk], writes=[f"gates{2 * ep + e2}.{b}"])


def stage_FF(k):
    S, nc = k.S, k.nc
    with Phase(k, "FF") as ph:
        xgr = ph.ring(2, [128, 8, NB * CAP], BF16, "xg")
        w13r = ph.ring(3, [128, 2, 8, 512], BF16, "w13")
        w2r = ph.ring(2, [128, 16, D], BF16, "w2")
        hid = ph.tile([128, 16, NB * CAP], BF16, "hid")
        sar = ph.ring(2, [128, 512], F32, "sa")
        ysr = ph.ring(2, [128, D], BF16, "ys")
        for e_ in range(NE):
            xg, xgk = xgr.next()
            S.dma('sp', xg[:], k.XG[e_], reads=[f"XG{e_}"], writes=[xgk])
            w1v = k.exp_w1[e_].rearrange("(k p) n -> p k n", p=128)
            w3v = k.exp_w3[e_].rearrange("(k p) n -> p k n", p=128)
            w2v = k.exp_w2[e_].rearrange("(k p) n -> p k n", p=128)
            w2, w2k = w2r.next()
            for fc in range(4):
                w13, w13k = w13r.next()
                S.dma('pool', w13[:, 0, :, :], w1v[:, :, fc * 512:(fc + 1) * 512], writes=[w13k + ".0"])
                S.dma('pool', w13[:, 1, :, :], w3v[:, :, fc * 512:(fc + 1) * 512], writes=[w13k + ".1"])
                S.dma('pool', w2[:, fc * 4:(fc + 1) * 4, :], w2v[:, fc * 4:(fc + 1) * 4, :], writes=[w2k + f".{fc}"])
                for fi in range(4):
                    fidx = fc * 4 + fi
                    ba = (fidx % 2) * 2
                    for mi in range(2):
                        mmgroup(S, k.bank(ba + mi), [(w13[:, mi, kk, fi * 128:(fi + 1) * 128], xg[:, kk, :]) for kk in range(8)],
                                reads=[w13k + f".{mi}", xgk], wkeys=[f"ps{ba + mi}"])
                    sa, sak = sar.next()
                    S.op('act', lambda e: e.activation(sa[:], k.bank(ba), AF.Silu), reads=[f"ps{ba}"], writes=[sak])
                    S.op('dve', lambda e: e.tensor_tensor(hid[:, fidx, :], sa[:], k.bank(ba + 1), ALU.mult), reads=[sak, f"ps{ba + 1}"], writes=[f"hid{fidx}"])
            hidk = [f"hid{i}" for i in range(16)]
            for sc in range(4):
                b, ch = sc // 2, sc % 2
                ys, ysk = ysr.next()
                for dc in range(2):
                    bk = 4 + (sc * 2 + dc) % 4
                    mmgroup(S, k.bank(bk), [(hid[:, f, sc * 128:(sc + 1) * 128], w2[:, f, dc * 512:(dc + 1) * 512]) for f in range(16)],
                            reads=hidk + [w2k + f".{i}" for i in range(4)], wkeys=[f"ps{bk}"])
                    if dc == 0:
                        S.op('act', lambda e: e.activation(ys[:, 0:512], k.bank(bk), AF.Copy, scale=k.gates[:, e_, b, ch:ch + 1]),
                             reads=[f"ps{bk}", f"gates{e_}.{b}"], writes=[ysk + ".0"])
                    else:
                        S.op('dve', lambda e: e.tensor_single_scalar(ys[:, 512:1024], k.bank(bk), k.gates[:, e_, b, ch:ch + 1], ALU.mult),
                             reads=[f"ps{bk}", f"gates{e_}.{b}"], writes=[ysk + ".1"])
                S.dma('sp', k.Y[b][:, e_ * 2 + ch, :], ys[:], reads=[ysk + ".0", ysk + ".1"], writes=[f"Y{b}"])


def stage_SC(k):
    S, nc = k.S, k.nc
    KW = int(os.environ.get("SC_WIN", "7"))
    with Phase(k, "SC") as ph:
        Yb = ph.tile([128, 2 * NE, D], BF16, "Yb")
        G2 = [load_mod(k, ph, b, 5, f"G2_{b}") for b in range(NB)]
        fg = ph.tile([128, D], F32, "fg")
        S.dma('sp', fg[:], bc_row(k.final_g, D), writes=["fg"])
        Dr = ph.ring(3, [128, NE, 128], BF16, "D")
        PTr = ph.ring(5, [128, 2, NE, 128], BF16, "PT")
        xmr = ph.ring(6, [128, D], F32, "xm")
        ttr = ph.ring(6, [128, D], F32, "tt")
        junk = ph.tile([128, D], BF16, "junk")
        sqr = ph.ring(KW, [128, 4], F32, "sq")
        outr = ph.ring(2, [128, D], F32, "out")
        identb, onesb = k.cbf["ident"], k.cbf["ones"]
        iotap = k.smallt[:, 256:258]
        vT = k.psum[:, 0:2048].rearrange("p (e t) -> p e t", e=NE)
        cnt = [0]

        def unit(b, blk, Ybk):
            r0 = b * L + blk * 128
            xm, xmk = xmr.next()
            S.dma('sp', xm[:], k.Xmid[r0:r0 + 128, :], reads=[f"Xmid{b}.{blk}"], writes=[xmk])
            Dt, Dk = Dr.next()
            S.op('dve', lambda e: e.tensor_tensor(Dt[:], identb[:].unsqueeze(1).to_broadcast([128, NE, 128]),
                                                  k.val[:, b, blk, :].unsqueeze(2).to_broadcast([128, NE, 128]), ALU.mult),
                 reads=["val", "identbf"], writes=[Dk])
            yield
            Dflat = Dt[:].rearrange("p e t -> p (e t)")

            def vtf(e):
                inst = None
                for c4 in range(4):
                    inst = e.matmul(k.bank(c4), onesb[:], Dflat[:, c4 * 512:(c4 + 1) * 512], start=True, stop=True)
                return inst
            S.op('pe', vtf, reads=[Dk, "onesbf"], writes=["ps0", "ps1", "ps2", "ps3"])
            PT, PTk = PTr.next()
            for ch in range(2):
                S.op('dve', lambda e: e.tensor_single_scalar(PT[:, ch, :, :], vT, iotap[:, ch:ch + 1], ALU.is_equal),
                     reads=["ps0", "ps1", "ps2", "ps3", "small"], writes=[PTk + f".{ch}"])
            yield
            yield
            yield
            par = cnt[0] % 2
            cnt[0] += 1
            b0 = 4 + 2 * par
            for dc in range(2):
                bk = b0 + dc
                mmgroup(S, k.bank(bk), [(PT[:, ch, e_, :], Yb[:, e_ * 2 + ch, dc * 512:(dc + 1) * 512]) for e_ in range(NE) for ch in range(2)],
                        reads=Ybk + [PTk + ".0", PTk + ".1"], wkeys=[f"ps{bk}"])
            tt, ttk = ttr.next()
            S.op('dve', lambda e: e.tensor_tensor(tt[:], k.psum[:, b0 * 512:(b0 + 2) * 512], G2[b][:], ALU.mult),
                 reads=[f"ps{b0}", f"ps{b0 + 1}", f"G2_{b}"], writes=[ttk])
            yield
            S.op('pool', lambda e: e.tensor_tensor(tt[:], tt[:], xm[:], ALU.add), reads=[ttk, xmk], writes=[ttk])
            yield
            sq, sqk = sqr.next()
            S.op('act', lambda e: e.activation(junk[:], tt[:], AF.Square, accum_out=sq[:, 0:1]), reads=[ttk], writes=["SC.junk", sqk + "a"])
            yield
            S.op('act', lambda e: e.activation(sq[:, 1:2], sq[:, 0:1], AF.Sqrt, scale=1.0 / D, bias=EPS), reads=[sqk + "a"], writes=[sqk + "b"])
            yield
            S.op('dve', lambda e: e.reciprocal(sq[:, 2:3], sq[:, 1:2]), reads=[sqk + "b"], writes=[sqk])
            ot, otk = outr.next()
            S.op('dve', lambda e: e.scalar_tensor_tensor(ot[:], tt[:], sq[:, 2:3], fg[:], ALU.mult, ALU.mult), reads=[ttk, sqk, "fg"], writes=[otk])
            S.dma('pool', k.out[r0:r0 + 128, :], ot[:], reads=[otk], writes=[f"out{b}.{blk}"])

        for b in range(NB):
            for q in range(8):
                S.dma('sp', Yb[:, q * 4:(q + 1) * 4, :], k.Y[b][:, q * 4:(q + 1) * 4, :], reads=[f"Y{b}"], writes=[f"Yb{q}"])
            Ybk = [f"Yb{q}" for q in range(8)]
            run_window((unit(b, blk, Ybk) for blk in range(16)), KW)


_NC_CACHE = {}


def make_in_maps(inputs):
    c = make_consts()
    f32 = lambda a: np.ascontiguousarray(np.asarray(a, dtype=np.float32))
    x = f32(inputs["x"])
    cc = f32(inputs["c"])
    ctx = f32(inputs["ctx"])
    c_ctx = f32(inputs["c_ctx"])
    shared = {
        "ada_w": f32(inputs["ada_w"])[0], "ada_b": f32(inputs["ada_b"]).reshape(1, 6 * D),
        "norm1_g": f32(inputs["norm1_g"]).reshape(1, D), "w_in": f32(inputs["w_in"])[0],
        "hy_conv_w": f32(inputs["hy_conv_w"])[0], "hy_conv_b": f32(inputs["hy_conv_b"]).reshape(1, 1536),
        "hy_pos_w1": f32(inputs["hy_pos_w1"])[0], "hy_pos_b1": f32(inputs["hy_pos_b1"]).reshape(64, 1),
        "hy_freq1": f32(inputs["hy_freq1"]).reshape(64, 1), "hy_pos_w2": f32(inputs["hy_pos_w2"])[0],
        "hy_pos_b2": f32(inputs["hy_pos_b2"]).reshape(64, 1), "hy_freq2": f32(inputs["hy_freq2"]).reshape(64, 1),
        "hy_pos_w3": f32(inputs["hy_pos_w3"])[0], "hy_bias": f32(inputs["hy_bias"])[0],
        "hy_norm_g": f32(inputs["hy_norm_g"]).reshape(1, 512), "hg_lb": f32(inputs["hg_lb"]).reshape(2, 1024),
        "hg_norm_g": f32(inputs["hg_norm_g"]).reshape(1, 128), "w_out": f32(inputs["w_out"])[0],
        "norm2_g": f32(inputs["norm2_g"]).reshape(1, D), "router_w": f32(inputs["router_w"])[0],
        "exp_w1": f32(inputs["exp_w1"])[0], "exp_w3": f32(inputs["exp_w3"])[0], "exp_w2": f32(inputs["exp_w2"])[0],
        "final_g": f32(inputs["final_g"]).reshape(1, D),
        "tabF": c["tabF"].reshape(2, 16, 128, 2048), "tabI": c["tabI"].reshape(2, 16, 128, 2048),
        "zT": c["zT"], "win": c["win"], "m128": c["m128"], "sel": c["sel"], "small": c["small"],
    }
    maps = []
    for i in range(NCORES):
        cl = np.stack([cc[NB * i], cc[NB * i + 1], c_ctx], axis=-1)
        clay = np.ascontiguousarray(cl.reshape(8, 128, 3).transpose(1, 0, 2))
        m = dict(shared)
        m["x"] = np.ascontiguousarray(x[NB * i:NB * i + NB].reshape(NB * L, D))
        m["ctx"] = np.ascontiguousarray(ctx[NB * i:NB * i + NB].reshape(NB * CTX, D))
        m["clay"] = clay
        maps.append(m)
    return maps


def kernel(**inputs):
    if "nc" not in _NC_CACHE:
        _NC_CACHE["nc"] = build_nc()[0]
    nc = _NC_CACHE["nc"]
    maps = make_in_maps(inputs)
    res = run_bass_kernel_spmd(nc, maps, core_ids=list(range(NCORES)))
    outs = [np.asarray(r["out"], dtype=np.float32).reshape(NB, L, D) for r in res.results]
    return np.concatenate(outs, axis=0)
```
